# Optimizing a Trainium2 kernel written in Bass

```python
import math
import jax, jax.numpy as jnp
from jax import lax
import numpy as np

D_MODEL = 1024
BATCH = 8
SEQ = 2048
DEPTH = 4

CONV_DIM = D_MODEL
CONV_KERNEL = 31
RET_QK_DIM = 256
RET_V_DIM = 512
RET_HEADS = D_MODEL // RET_QK_DIM
RET_QK_WIDTH = RET_HEADS * RET_QK_DIM
RET_V_WIDTH = RET_HEADS * RET_V_DIM
RET_CHUNK = 128
ROPE_BASE = 10000.0
N_BRANCHES = 2
IN_SIZES = (CONV_DIM, CONV_DIM, RET_QK_WIDTH, RET_QK_WIDTH, RET_V_WIDTH, RET_V_WIDTH, D_MODEL, D_MODEL)
IN_WIDTH = sum(IN_SIZES)
IN_SPLITS = tuple(int(s) for s in np.cumsum(IN_SIZES)[:-1])
D_FF = 2816
N_EXPERTS = 8
TOP_K = 2
N_DENSE = (DEPTH + 1) // 2
N_MOE = DEPTH // 2
DEEPNORM_ALPHA = (2 * DEPTH) ** 0.25
DEEPNORM_BETA = (8 * DEPTH) ** -0.25
N_ADA = 6
LN_EPS = 1e-5

kernel_name = "hybrid_conv_retention_moe_deepnorm_adaln"


def layer_norm(x, g, b):
    xf = x.astype(jnp.float32)
    mu = jnp.mean(xf, axis=-1, keepdims=True)
    var = jnp.mean(jnp.square(xf - mu), axis=-1, keepdims=True)
    y = (xf - mu) * lax.rsqrt(var + LN_EPS)
    return (y * g.astype(jnp.float32) + b.astype(jnp.float32)).astype(x.dtype)


def head_norm(x):
    mu = jnp.mean(x, axis=-1, keepdims=True)
    var = jnp.mean(jnp.square(x - mu), axis=-1, keepdims=True)
    return (x - mu) * lax.rsqrt(var + LN_EPS)


def rotary(x, positions):
    half = x.shape[-1] // 2
    inv_freq = ROPE_BASE ** (-jnp.arange(half, dtype=jnp.float32) / half)
    ang = positions.astype(jnp.float32)[:, :, None] * inv_freq
    cos = jnp.cos(ang)[:, :, None, :]
    sin = jnp.sin(ang)[:, :, None, :]
    xf = x.astype(jnp.float32)
    x1, x2 = xf[..., :half], xf[..., half:]
    return jnp.concatenate([x1 * cos - x2 * sin, x2 * cos + x1 * sin], axis=-1)


def chunkwise_retention(q, k, v):
    b, t, h, dk = q.shape
    dv = v.shape[-1]
    n_chunks = t // RET_CHUNK
    log_g = jnp.log1p(-jnp.exp2(-5.0 - jnp.arange(h, dtype=jnp.float32)))
    j = jnp.arange(RET_CHUNK, dtype=jnp.float32)
    diff = j[:, None] - j[None, :]
    intra = jnp.where(diff >= 0, jnp.exp(log_g[:, None, None] * jnp.maximum(diff, 0.0)), 0.0)
    q_decay = jnp.exp(log_g[:, None] * (j + 1.0))[None, :, :, None]
    k_decay = jnp.exp(log_g[:, None] * (RET_CHUNK - 1.0 - j))[None, :, :, None]
    chunk_decay = jnp.exp(log_g * RET_CHUNK)[None, :, None, None]

    def to_chunks(a):
        return a.reshape(b, n_chunks, RET_CHUNK, h, a.shape[-1]).transpose(1, 0, 3, 2, 4)

    qc, kc, vc = to_chunks(q), to_chunks(k * (dk ** -0.5)), to_chunks(v)

    def step(state, inp):
        qi, ki, vi = inp
        scores = jnp.einsum('bhid,bhjd->bhij', qi, ki) * intra
        inner = jnp.einsum('bhij,bhjv->bhiv', scores, vi)
        cross = jnp.einsum('bhid,bhdv->bhiv', qi * q_decay, state)
        new_state = state * chunk_decay + jnp.einsum('bhjd,bhjv->bhdv', ki * k_decay, vi)
        return new_state, inner + cross

    state0 = jnp.zeros((b, h, dk, dv), jnp.float32)
    _, out = lax.scan(step, state0, (qc, kc, vc))
    return out.transpose(1, 0, 3, 2, 4).reshape(b, t, h, dv)


def causal_depthwise_conv(u, w, bias):
    y = lax.conv_general_dilated(
        u, w[:, None, :].astype(u.dtype), window_strides=(1,), padding=[(CONV_KERNEL - 1, 0)],
        dimension_numbers=('NWC', 'WIO', 'NWC'), feature_group_count=u.shape[-1])
    return y + bias


def token_mixer(h, positions, w_in, w_dw, b_dw, ln_conv_g, ln_conv_b, w_conv_o, w_ret_o, w_out):
    b, t, _ = h.shape
    proj = h @ w_in
    conv_a, conv_b, q, k, v, ret_gate, gate_conv, gate_ret = jnp.split(proj, IN_SPLITS, axis=-1)
    u = conv_a * jax.nn.sigmoid(conv_b)
    u = causal_depthwise_conv(u, w_dw, b_dw)
    u = jax.nn.silu(layer_norm(u, ln_conv_g, ln_conv_b))
    y_conv = u @ w_conv_o
    q = rotary(q.reshape(b, t, RET_HEADS, RET_QK_DIM), positions)
    k = rotary(k.reshape(b, t, RET_HEADS, RET_QK_DIM), positions)
    v = v.reshape(b, t, RET_HEADS, RET_V_DIM).astype(jnp.float32)
    o = head_norm(chunkwise_retention(q, k, v)).reshape(b, t, RET_V_WIDTH).astype(h.dtype)
    y_ret = (jax.nn.silu(ret_gate) * o) @ w_ret_o
    merged = jax.nn.sigmoid(gate_conv) * y_conv + jax.nn.sigmoid(gate_ret) * y_ret
    return merged @ w_out


def swiglu(h, w_gate, w_up, w_down):
    return (jax.nn.silu(h @ w_gate) * (h @ w_up)) @ w_down


def moe_swiglu(h, w_router, w_gate, w_up, w_down):
    b, t, d = h.shape
    tokens = h.reshape(b * t, d)
    logits = (tokens @ w_router).astype(jnp.float32)
    top_vals, top_idx = lax.top_k(logits, TOP_K)
    top_w = jax.nn.softmax(top_vals, axis=-1)
    combine = jnp.sum(jax.nn.one_hot(top_idx, N_EXPERTS, dtype=jnp.float32) * top_w[..., None], axis=1)
    combine = combine.astype(tokens.dtype)
    out = jnp.zeros_like(tokens)
    for e in range(N_EXPERTS):
        out = out + combine[:, e:e + 1] * swiglu(tokens, w_gate[e], w_up[e], w_down[e])
    return out.reshape(b, t, d)


def setup_inputs(seed: int = 0) -> dict:
    key = jax.random.key(seed)
    ks = jax.random.split(key, 32)
    f32 = jnp.float32
    D = D_MODEL

    def nrm(k, shape, scale):
        return jax.random.normal(k, shape, f32) * scale

    x = jax.random.normal(ks[0], (BATCH, SEQ, D), f32)
    c = jax.random.normal(ks[1], (BATCH, D), f32)
    positions = (jnp.arange(SEQ, dtype=jnp.int32)[None, :]
                 + jax.random.randint(ks[2], (BATCH, 1), 0, 1024, dtype=jnp.int32))
    return {
        "x": x,
        "c": c,
        "positions": positions,
        "w_ada": nrm(ks[3], (DEPTH, D, N_ADA * D), D ** -0.5),
        "b_ada": nrm(ks[4], (DEPTH, N_ADA * D), 0.02),
        "w_in": nrm(ks[5], (DEPTH, D, IN_WIDTH), D ** -0.5),
        "w_dw": nrm(ks[6], (DEPTH, CONV_KERNEL, CONV_DIM), CONV_KERNEL ** -0.5),
        "b_dw": nrm(ks[7], (DEPTH, CONV_DIM), 0.02),
        "ln_conv_g": 1.0 + nrm(ks[8], (DEPTH, CONV_DIM), 0.02),
        "ln_conv_b": nrm(ks[9], (DEPTH, CONV_DIM), 0.02),
        "w_conv_o": nrm(ks[10], (DEPTH, CONV_DIM, D), CONV_DIM ** -0.5),
        "w_ret_o": nrm(ks[11], (DEPTH, RET_V_WIDTH, D), RET_V_WIDTH ** -0.5),
        "w_out": nrm(ks[12], (DEPTH, D, D), DEEPNORM_BETA * D ** -0.5),
        "ln1_g": 1.0 + nrm(ks[13], (DEPTH, D), 0.02),
        "ln1_b": nrm(ks[14], (DEPTH, D), 0.02),
        "ffn_w_gate": nrm(ks[15], (N_DENSE, D, D_FF), D ** -0.5),
        "ffn_w_up": nrm(ks[16], (N_DENSE, D, D_FF), D ** -0.5),
        "ffn_w_down": nrm(ks[17], (N_DENSE, D_FF, D), DEEPNORM_BETA * D_FF ** -0.5),
        "moe_w_router": nrm(ks[18], (N_MOE, D, N_EXPERTS), D ** -0.5),
        "moe_w_gate": nrm(ks[19], (N_MOE, N_EXPERTS, D, D_FF), D ** -0.5),
        "moe_w_up": nrm(ks[20], (N_MOE, N_EXPERTS, D, D_FF), D ** -0.5),
        "moe_w_down": nrm(ks[21], (N_MOE, N_EXPERTS, D_FF, D), DEEPNORM_BETA * D_FF ** -0.5),
        "ln2_g": 1.0 + nrm(ks[22], (DEPTH, D), 0.02),
        "ln2_b": nrm(ks[23], (DEPTH, D), 0.02),
    }


def reference(x, c, positions, w_ada, b_ada, w_in, w_dw, b_dw, ln_conv_g, ln_conv_b,
              w_conv_o, w_ret_o, w_out, ln1_g, ln1_b, ffn_w_gate, ffn_w_up, ffn_w_down,
              moe_w_router, moe_w_gate, moe_w_up, moe_w_down, ln2_g, ln2_b):
    c_act = jax.nn.silu(c)
    for l in range(DEPTH):
        ada = c_act @ w_ada[l] + b_ada[l]
        shift1, scale1, gate1, shift2, scale2, gate2 = [a[:, None, :] for a in jnp.split(ada, N_ADA, axis=-1)]
        h = x * (1.0 + scale1) + shift1
        mix = token_mixer(h, positions, w_in[l], w_dw[l], b_dw[l], ln_conv_g[l], ln_conv_b[l],
                          w_conv_o[l], w_ret_o[l], w_out[l])
        x = layer_norm(DEEPNORM_ALPHA * x + gate1 * mix, ln1_g[l], ln1_b[l])
        h = x * (1.0 + scale2) + shift2
        if l % 2 == 0:
            i = l // 2
            ff = swiglu(h, ffn_w_gate[i], ffn_w_up[i], ffn_w_down[i])
        else:
            i = l // 2
            ff = moe_swiglu(h, moe_w_router[i], moe_w_gate[i], moe_w_up[i], moe_w_down[i])
        x = layer_norm(DEEPNORM_ALPHA * x + gate2 * ff, ln2_g[l], ln2_b[l])
    return x
```

```python
import numpy as np
from contextlib import ExitStack
import concourse.bass as bass
import concourse.mybir as mybir
from concourse.bass_utils import run_bass_kernel_spmd

F32 = mybir.dt.float32
BF16 = mybir.dt.bfloat16
I32 = mybir.dt.int32
AF = mybir.ActivationFunctionType
ALU = mybir.AluOpType

DEPTH = 4
D = 1024
T = 2048
NKC = 8
GT = 512
NG = T // GT
DFF = 2816
NFC = DFF // 128
NE = 8
HEADS = 4
DK = 256
DV = 512
CK = 31
IN_W = 10240
ALPHA = float((2 * DEPTH) ** 0.25)
EPS = 1e-5
P_BDW, P_LNCG, P_LNCB, P_LN1G, P_LN1B, P_LN2G, P_LN2B, P_WDW, P_BADA = 0, 1, 2, 3, 4, 5, 6, 7, 38
NP_ = 44
V_S1, V_SH1, V_G1, V_XS1, V_XB1, V_GS2, V_BS2, V_G2, V_XS2, V_XB2, V_TMP = range(11)
NV = 11
C_ID, C_ONES, C_MASK, C_QDEC, C_KDEC, C_INVF, C_END = 0, 128, 256, 768, 1280, 1284, 1285
NRING = 4
FB = 4


class Eng:
    def __init__(self, name, e, sem):
        self.name, self.e, self.sem, self.n, self.known = name, e, sem, 0, {}


class Buf:
    def __init__(self, t):
        self.t = t
        self.w = None
        self.r = {}
        self.dsem = None
        self.dn = 0

    def __getitem__(self, idx):
        return self.t[idx]


class Ctx:
    def __init__(self, nc, es):
        self.nc, self.es, self.dry = nc, es, False
        self.sems = {}
        self.eng = {}
        for name, e in [("pe", nc.tensor), ("act", nc.scalar), ("dve", nc.vector),
                        ("pool", nc.gpsimd), ("sp", nc.sync)]:
            sem = es.enter_context(nc.semaphore("s_" + name))
            self.sems[sem.num] = sem
            self.eng[name] = Eng(name, e, sem)
        self.uid = 0
        self.dpool = []
        for i in range(44):
            sem = es.enter_context(nc.semaphore(f"d{i}"))
            self.sems[sem.num] = sem
            self.dpool.append(sem)
        self.dpool_sw = []
        for i in range(8):
            sem = es.enter_context(nc.semaphore(f"w{i}"))
            self.sems[sem.num] = sem
            self.dpool_sw.append(sem)
        self.dcount = {}
        self.psb = [Buf(es.enter_context(nc.psum_tensor(f"psb{i}", [128, 512], F32))) for i in range(8)]
        self.psi = 0
        self.ps_pool = list(range(8))

    def sb(self, shape, dt, st=None, name=None):
        self.uid += 1
        st = st or self.es
        b = Buf(st.enter_context(self.nc.sbuf_tensor(f"{name or 'b'}{self.uid}", shape, dt)))
        b.st = st
        return b

    def dram(self, name, shape, dt, kind=None):
        if kind:
            return Buf(self.nc.dram_tensor(name, shape, dt, kind=kind))
        return Buf(self.nc.dram_tensor(name, shape, dt))

    def ps(self):
        pool = self.ps_pool
        b = self.psb[pool[self.psi % len(pool)]]
        self.psi += 1
        return b

    def _need(self, reads, writes):
        need = {}

        def add(k, v):
            if need.get(k, 0) < v:
                need[k] = v
        for b in reads:
            if b.w:
                add(*b.w)
        for b in writes:
            if b.w:
                add(*b.w)
            for k, v in b.r.items():
                add(k, v)
        return need

    def _wait(self, E, need):
        for k, v in need.items():
            if k == E.sem.num and E.name == "pe":
                continue
            if E.known.get(k, 0) >= v:
                continue
            E.e.wait_ge(self.sems[k], v)
            E.known[k] = v

    def op(self, eng, fn, reads=(), writes=(), inc=True):
        if self.dry:
            return None
        E = self.eng[eng]
        self._wait(E, self._need(reads, writes))
        ins = fn(E.e)
        if inc:
            E.n += 1
            ins.then_inc(E.sem, 1)
            tick = (E.sem.num, E.n)
        else:
            tick = (E.sem.num, E.n + 1)
        for b in reads:
            if b.r.get(tick[0], 0) < tick[1]:
                b.r[tick[0]] = tick[1]
        for b in writes:
            b.w = tick
            b.r = {}
        return ins

    def dma(self, eng, pairs, reads=(), writes=(), **kw):
        if self.dry:
            return
        E = self.eng[eng]
        self._wait(E, self._need(reads, writes))
        anchor = writes[0] if writes else reads[0]
        if anchor.dsem is None:
            pool = self.dpool_sw if eng == "pool" else self.dpool
            anchor.dsem = pool.pop(0)
            st = getattr(anchor, "st", None)
            if st is not None and st is not self.es:
                st.callback(lambda sem=anchor.dsem, pool=pool: pool.append(sem))
        num = anchor.dsem.num
        for o, i in pairs:
            E.e.dma_start(out=o, in_=i, **kw).then_inc(anchor.dsem, 16)
            self.dcount[num] = self.dcount.get(num, 0) + 16
        tick = (num, self.dcount[num])
        for b in reads:
            if b.r.get(tick[0], 0) < tick[1]:
                b.r[tick[0]] = tick[1]
        for b in writes:
            b.w = tick
            b.r = {}

    def barrier(self, engs=("pe", "act", "dve", "sp"), bufs=()):
        if self.dry:
            return
        for a in engs:
            A = self.eng[a]
            self._wait(A, self._need((), bufs))
            for b in ("pe", "act", "dve"):
                if a == b:
                    continue
                B = self.eng[b]
                if B.n > A.known.get(B.sem.num, 0):
                    A.e.wait_ge(B.sem, B.n)
                    A.known[B.sem.num] = B.n

    def snapshot(self):
        return {n: self.eng[n].n for n in ("pe", "act", "dve")}

    def barrier_to(self, engs, snap):
        if self.dry:
            return
        for a in engs:
            A = self.eng[a]
            for b, v in snap.items():
                if a == b:
                    continue
                B = self.eng[b]
                if v > A.known.get(B.sem.num, 0):
                    A.e.wait_ge(B.sem, v)
                    A.known[B.sem.num] = v

    def wait_buf(self, eng, b):
        if self.dry:
            return
        E = self.eng[eng]
        need = {}
        if b.w:
            need[b.w[0]] = b.w[1]
        self._wait(E, need)


class WStream:
    def __init__(self, k, n):
        self.k, self.n = k, n
        self.slabs = [k.sb([128, 4096], BF16, name="wr") for _ in range(n)]
        self.reqs = []
        self.i = self.loaded = self.released = 0

    def get(self, spec):
        if self.k.dry:
            self.reqs.append(spec)
            return self.slabs[0]
        idx = self.i
        self.i += 1
        self._pump()
        assert self.loaded > idx, "weight ring too small for holding pattern"
        return self.slabs[idx % self.n]

    def release(self, cnt=1):
        if self.k.dry:
            return
        self.released += cnt
        self._pump()

    def _pump(self):
        while self.loaded < len(self.reqs) and self.loaded < self.released + self.n:
            j = self.loaded
            slab = self.slabs[j % self.n]
            pairs = [(fn(slab.t), src) for fn, src in self.reqs[j]]
            self.k.dma("pool", pairs, writes=[slab])
            self.loaded += 1

    def reset(self):
        self.i = self.loaded = self.released = 0


def build_program(layers, fused):
    nc = bass.Bass("TRN2", target_bir_lowering=False)
    NL = len(layers)

    def din(name, shape, dt=F32):
        return nc.dram_tensor(name, shape, dt, kind="ExternalInput").ap()

    LW = DEPTH if fused else 1
    xT = din("xT", [D, T])
    cT = din("cT", [128, NKC])
    posb = din("posb", [128, T], I32)
    params = din("params", [128, LW * NKC * NP_])
    consts = din("consts", [128, C_END])
    w_ada = din("w_ada", [LW, D, 6 * D])
    w_in = din("w_in", [LW, D, IN_W])
    w_conv_o = din("w_conv_o", [LW, D, D])
    w_ret_o = din("w_ret_o", [LW, 2 * D, D])
    w_out = din("w_out", [LW, D, D])
    has_dense = any(l % 2 == 0 for l in layers)
    has_moe = any(l % 2 == 1 for l in layers)
    ND = 2 if fused else 1
    if has_dense:
        ffn_wg = din("ffn_w_gate", [ND, D, DFF])
        ffn_wu = din("ffn_w_up", [ND, D, DFF])
        ffn_wd = din("ffn_w_down", [ND, DFF, D])
    if has_moe:
        moe_wr = din("moe_wr", [128, ND * NKC * NE])
        moe_wg = din("moe_w_gate", [ND, NE, D, DFF])
        moe_wu = din("moe_w_up", [ND, NE, D, DFF])
        moe_wd = din("moe_w_down", [ND, NE, DFF, D])
    outT = nc.dram_tensor("outT", [D, T], F32, kind="ExternalOutput").ap()
    cs_scr = nc.dram_tensor("cs_scr", [2, 128, T], F32).ap()
    h2_scr = nc.dram_tensor("h2_scr", [NKC, 128, T], BF16).ap()

    with ExitStack() as es:
        k = Ctx(nc, es)
        OUTB = Buf(None)
        CSB = Buf(None)
        H2B = [Buf(None) for _ in range(NKC)]
        XA = [k.sb([128, T], F32, name="xa") for _ in range(NKC)]
        PARL = k.sb([128, NKC, NP_], F32, name="parl")
        CON = k.sb([128, C_END], F32, name="con")
        IDB = k.sb([128, 128], BF16, name="idb")
        ONB = k.sb([128, 128], BF16, name="onb")
        VEC = [k.sb([128, NV, NKC], F32, name="vec") for _ in range(NL)]
        CAB = k.sb([128, NKC], BF16, name="cab")
        ws = WStream(k, NRING)
        if has_moe:
            WRB = k.sb([128, ND, NKC, NE], BF16, name="wrb")

        ident_f = CON[:, C_ID:C_ID + 128]
        ones_f = CON[:, C_ONES:C_ONES + 128]

        def lw(l):
            return l if fused else 0

        def emit():
            k.dma("sp", [(CON[:], consts)], writes=[CON])
            for c in range(NKC):
                k.dma("sp", [(XA[c][:], xT[c * 128:(c + 1) * 128, :])], writes=[XA[c]])
            k.op("dve", lambda e: e.tensor_copy(out=IDB[:], in_=ident_f), reads=[CON], writes=[IDB])
            k.op("dve", lambda e: e.tensor_copy(out=ONB[:], in_=ones_f), reads=[CON], writes=[ONB])
            with ExitStack() as ph:
                PAR = k.sb([128, LW, NKC, NP_], F32, ph)
                k.dma("sp", [(PAR[:].rearrange("p l c n -> p (l c n)"), params)], writes=[PAR])
                if has_moe:
                    WRT = k.sb([128, ND, NKC, NE], F32, ph)
                    k.dma("sp", [(WRT[:].rearrange("p l c n -> p (l c n)"), moe_wr)], writes=[WRT])
                    k.op("dve", lambda e: e.tensor_copy(out=WRB[:], in_=WRT[:]), reads=[WRT], writes=[WRB])
                CT = k.sb([128, NKC], F32, ph)
                CA = k.sb([128, NKC], F32, ph)
                ADA = k.sb([128, 48], F32, ph)
                WA = [k.sb([128, NKC, 512], BF16, ph) for _ in range(3)]
                k.dma("sp", [(CT[:], cT)], writes=[CT])
                k.op("act", lambda e: e.activation(out=CA[:], in_=CT[:], func=AF.Silu), reads=[CT], writes=[CA])
                POSI = k.sb([128, T], I32, ph)
                k.dma("sp", [(POSI[:], posb)], writes=[POSI])
                sc = [k.sb([128, GT], F32, ph) for _ in range(4)]
                sci = k.sb([128, GT], I32, ph)
                invf = CON[:, C_INVF:C_INVF + 1]
                for g in range(NG):
                    cols = slice(g * GT, (g + 1) * GT)
                    ang, t_, tf, r_ = sc
                    k.op("dve", lambda e: e.tensor_copy(out=ang[:], in_=POSI[:, cols]), reads=[POSI], writes=[ang])
                    k.op("dve", lambda e: e.tensor_scalar(out=ang[:], in0=ang[:], scalar1=invf, scalar2=None, op0=ALU.mult),
                         reads=[ang, CON], writes=[ang])
                    for which, off in ((0, 0.75), (1, 0.5)):
                        k.op("dve", lambda e, off=off: e.tensor_scalar(out=t_[:], in0=ang[:], scalar1=float(1.0 / (2 * np.pi)),
                                                                       scalar2=off, op0=ALU.mult, op1=ALU.add),
                             reads=[ang], writes=[t_])
                        k.op("dve", lambda e: e.tensor_copy(out=sci[:], in_=t_[:]), reads=[t_], writes=[sci])
                        k.op("dve", lambda e: e.tensor_copy(out=tf[:], in_=sci[:]), reads=[sci], writes=[tf])
                        k.op("dve", lambda e: e.tensor_tensor(out=t_[:], in0=t_[:], in1=tf[:], op=ALU.subtract),
                             reads=[t_, tf], writes=[t_])
                        k.op("dve", lambda e: e.tensor_scalar(out=tf[:], in0=t_[:], scalar1=0.0, scalar2=None, op0=ALU.is_lt),
                             reads=[t_], writes=[tf])
                        k.op("dve", lambda e: e.tensor_tensor(out=t_[:], in0=t_[:], in1=tf[:], op=ALU.add),
                             reads=[t_, tf], writes=[t_])
                        k.op("dve", lambda e: e.tensor_scalar(out=tf[:], in0=t_[:], scalar1=1.0, scalar2=None, op0=ALU.is_ge),
                             reads=[t_], writes=[tf])
                        k.op("dve", lambda e: e.tensor_tensor(out=t_[:], in0=t_[:], in1=tf[:], op=ALU.subtract),
                             reads=[t_, tf], writes=[t_])
                        k.op("dve", lambda e: e.tensor_scalar(out=r_[:], in0=t_[:], scalar1=6.283185, scalar2=-3.1415925,
                                                              op0=ALU.mult, op1=ALU.add), reads=[t_], writes=[r_])
                        k.op("act", lambda e: e.activation(out=r_[:], in_=r_[:], func=AF.Sin), reads=[r_], writes=[r_])
                        k.dma("sp", [(cs_scr[which, :, cols], r_[:])], reads=[r_], writes=[CSB])
                si = 0
                k.op("dve", lambda e: e.tensor_copy(out=CAB[:], in_=CA[:]), reads=[CA], writes=[CAB])
                for li, l in enumerate(layers[:1]):
                    wa = w_ada[lw(l)].rearrange("(kc p) m -> p kc m", p=128)
                    pa = k.ps()
                    for s in range(12):
                        slab = WA[si % 3]
                        si += 1
                        k.dma("pool", [(slab[:], wa[:, :, s * 512:(s + 1) * 512])], writes=[slab])
                        for j in range(4):
                            oc = s * 4 + j
                            for kc in range(NKC):
                                k.op("pe", lambda e, slab=slab, j=j, kc=kc, oc=oc: e.matmul(
                                    pa[:, oc:oc + 1], lhsT=slab[:, kc, j * 128:(j + 1) * 128], rhs=CAB[:, kc:kc + 1],
                                    start=(kc == 0), stop=(kc == NKC - 1)),
                                    reads=[slab, CAB], writes=[pa], inc=(kc == NKC - 1))
                    compute_vec(li, l, pa, ADA, PAR, lambda c, l=l: PAR[:, lw(l), :, c])
                for c in range(NKC):
                    k.op("act", lambda e, c=c: e.activation(out=XA[c][:], in_=XA[c][:], func=AF.Identity, scale=ALPHA), reads=[XA[c]], writes=[XA[c]])
                k.barrier(bufs=sc)


            import os
            stage = os.environ.get("MK_STAGE", "all")
            for li, l in enumerate(layers):
                if stage in ("all", "mixer"):
                    mixer(li, l)
                    k.barrier()
                if stage in ("all", "ffn"):
                    ffn(li, l)
                    k.barrier()
                    k.ps_pool = list(range(8))

            for c in range(NKC):
                k.dma("sp", [(outT[c * 128:(c + 1) * 128, :], XA[c][:])], reads=[XA[c]], writes=[OUTB])
            k.wait_buf("sp", OUTB)

        def compute_vec(li, l, pa, ADA, PARB, Pp):
            for j in range(6):
                k.op("dve", lambda e, j=j: e.tensor_tensor(
                    out=ADA[:, j * 8:(j + 1) * 8], in0=pa[:, j * 8:(j + 1) * 8],
                    in1=Pp(P_BADA + j), op=ALU.add), reads=[pa, PARB], writes=[ADA])
            V = VEC[li]
            last = (fused and l == DEPTH - 1)

            def vop(fn):
                k.op("dve", fn, reads=[ADA, PARB, V], writes=[V])
            A_ = lambda j: ADA[:, j * 8:(j + 1) * 8]
            vop(lambda e: e.tensor_scalar(out=V[:, V_S1, :], in0=A_(1), scalar1=1.0, scalar2=1.0 / ALPHA, op0=ALU.add, op1=ALU.mult))
            vop(lambda e: e.tensor_copy(out=V[:, V_SH1, :], in_=A_(0)))
            vop(lambda e: e.tensor_copy(out=V[:, V_G1, :], in_=A_(2)))
            vop(lambda e: e.tensor_scalar(out=V[:, V_XS1, :], in0=Pp(P_LN1G), scalar1=ALPHA, scalar2=None, op0=ALU.mult))
            vop(lambda e: e.tensor_scalar(out=V[:, V_XB1, :], in0=Pp(P_LN1B), scalar1=ALPHA, scalar2=None, op0=ALU.mult))
            vop(lambda e: e.tensor_scalar(out=V[:, V_TMP, :], in0=A_(4), scalar1=1.0, scalar2=None, op0=ALU.add))
            vop(lambda e: e.tensor_tensor(out=V[:, V_GS2, :], in0=Pp(P_LN1G), in1=V[:, V_TMP, :], op=ALU.mult))
            vop(lambda e: e.tensor_tensor(out=V[:, V_BS2, :], in0=Pp(P_LN1B), in1=V[:, V_TMP, :], op=ALU.mult))
            vop(lambda e: e.tensor_tensor(out=V[:, V_BS2, :], in0=V[:, V_BS2, :], in1=A_(3), op=ALU.add))
            vop(lambda e: e.tensor_copy(out=V[:, V_G2, :], in_=A_(5)))
            a2 = 1.0 if (last or not fused) else ALPHA
            vop(lambda e: e.tensor_scalar(out=V[:, V_XS2, :], in0=Pp(P_LN2G), scalar1=a2, scalar2=None, op0=ALU.mult))
            vop(lambda e: e.tensor_scalar(out=V[:, V_XB2, :], in0=Pp(P_LN2B), scalar1=a2, scalar2=None, op0=ALU.mult))

        def ln_stats(ph_scr, srcs, mu, rstd, bf_src):
            p1 = k.ps()
            n = len(srcs)
            for i, (b, ap) in enumerate(srcs):
                if bf_src:
                    k.op("pe", lambda e, ap=ap, i=i: e.matmul(p1[:], lhsT=ONB[:], rhs=ap, start=(i == 0), stop=(i == n - 1)),
                         reads=[b, ONB], writes=[p1])
                else:
                    xb = ph_scr()
                    xbv = xb[:].bitcast(BF16)[:, 0:GT]
                    if i % 2 == 0:
                        k.op("dve", lambda e, ap=ap, xbv=xbv: e.tensor_copy(out=xbv, in_=ap), reads=[b], writes=[xb])
                    else:
                        k.op("act", lambda e, ap=ap, xbv=xbv: e.activation(out=xbv, in_=ap, func=AF.Identity), reads=[b], writes=[xb])
                    k.op("pe", lambda e, xbv=xbv, i=i: e.matmul(p1[:], lhsT=ONB[:], rhs=xbv, start=(i == 0), stop=(i == n - 1)),
                         reads=[xb, ONB], writes=[p1])
            p2 = k.ps()
            for i, (b, ap) in enumerate(srcs):
                sq = ph_scr()
                sqv = sq[:].bitcast(BF16)[:, 0:GT]
                k.op("act", lambda e, ap=ap, sqv=sqv: e.activation(out=sqv, in_=ap, func=AF.Square), reads=[b], writes=[sq])
                k.op("pe", lambda e, sqv=sqv, i=i: e.matmul(p2[:], lhsT=ONB[:], rhs=sqv, start=(i == 0), stop=(i == n - 1)),
                     reads=[sq, ONB], writes=[p2])
            dn = float(1.0 / (128 * n))
            k.op("dve", lambda e: e.tensor_scalar(out=mu[:], in0=p1[:], scalar1=dn, scalar2=None, op0=ALU.mult),
                 reads=[p1], writes=[mu])
            k.op("dve", lambda e: e.tensor_tensor(out=rstd[:], in0=mu[:], in1=mu[:], op=ALU.mult), reads=[mu], writes=[rstd])
            k.op("dve", lambda e: e.scalar_tensor_tensor(out=rstd[:], in0=p2[:], scalar=dn, in1=rstd[:], op0=ALU.mult,
                                                          op1=ALU.subtract), reads=[p2, rstd], writes=[rstd])
            k.op("dve", lambda e: e.tensor_scalar(out=rstd[:], in0=rstd[:], scalar1=EPS, scalar2=None, op0=ALU.add),
                 reads=[rstd], writes=[rstd])
            k.op("act", lambda e: e.activation(out=rstd[:], in_=rstd[:], func=AF.Sqrt), reads=[rstd], writes=[rstd])
            k.op("dve", lambda e: e.reciprocal(out=rstd[:], in_=rstd[:]), reads=[rstd], writes=[rstd])
            return mu, rstd

        def slab_spec(wmat, c0, ncols, nk, dst_off=0, tot=None):
            tot = tot or ncols
            src = wmat.rearrange("(kc p) m -> p kc m", p=128)[:, :, c0:c0 + ncols]
            return (lambda t, nk=nk, tot=tot, dst_off=dst_off, ncols=ncols:
                    t[:, 0:nk * tot].rearrange("p (kc m) -> p kc m", kc=nk)[:, :, dst_off:dst_off + ncols], src)

        def wview(slab, nk, tot):
            return slab.t[:, 0:nk * tot].rearrange("p (kc m) -> p kc m", kc=nk)

        def proj_fm(slab, wv, j, H1, pb):
            for kc in range(NKC):
                k.op("pe", lambda e, kc=kc: e.matmul(pb[:], lhsT=wv[:, kc, j * 128:(j + 1) * 128], rhs=H1[kc][:],
                                                     start=(kc == 0), stop=(kc == NKC - 1)),
                     reads=[slab, H1[kc]], writes=[pb], inc=(kc == NKC - 1))

        def mixer(li, l):
            L = lw(l)
            V = VEC[li]
            win = w_in[L]
            k.dma("sp", [(PARL[:], params.rearrange("p (l c n) -> p l c n", l=LW, c=NKC)[:, L])], writes=[PARL])
            with ExitStack() as ph:
                H1 = [k.sb([128, GT], BF16, ph) for _ in range(NKC)]
                ST32 = [[k.sb([128, DV], F32, ph) for _ in range(2)] for _ in range(HEADS)]
                STB = [[k.sb([128, DV], BF16, ph) for _ in range(2)] for _ in range(2)]
                HALO = k.sb([128, NKC, CK - 1], BF16, ph)
                M = [k.sb([128, GT], BF16, ph) for _ in range(NKC)]
                scr = [k.sb([128, GT], F32, ph) for _ in range(4)]
                MU = k.sb([128, GT], F32, ph)
                RSTD = k.sb([128, GT], F32, ph)
                scr_i = [0]

                def scratch():
                    b = scr[scr_i[0] % len(scr)]
                    scr_i[0] += 1
                    return b
                for h in range(HEADS):
                    for dc in range(2):
                        k.op("dve", lambda e, h=h, dc=dc: e.memset(ST32[h][dc][:], 0.0), writes=[ST32[h][dc]])
                k.op("dve", lambda e: e.memset(HALO[:], 0.0), writes=[HALO])

                prev_snap, prev_staging = None, []
                for g in range(NG):
                    cols = slice(g * GT, (g + 1) * GT)
                    k.ps_pool = list(range(8))
                    if prev_snap is not None:
                        k.barrier_to(("pe", "act", "dve", "sp"), prev_snap)
                    def emit_h1(gg):
                        cc = slice(gg * GT, (gg + 1) * GT)
                        for kc in range(NKC):
                            k.op("act", lambda e, kc=kc, cc=cc: e.activation(out=H1[kc][:], in_=XA[kc][:, cc], func=AF.Identity,
                                                                            scale=V[:, V_S1, kc:kc + 1], bias=V[:, V_SH1, kc:kc + 1]),
                                 reads=[XA[kc], V], writes=[H1[kc]])
                    if g == 0:
                        emit_h1(0)
                    with ExitStack() as sp_:
                        U = [k.sb([128, CK - 1 + GT], BF16, sp_) for _ in range(NKC)]
                        CO = [k.sb([128, GT], BF16, sp_) for _ in range(NKC)]
                        DGs = [k.sb([128, CK, 128], BF16, sp_) for _ in range(2)]
                        SG = [k.sb([128, GT], BF16, sp_) for _ in range(NKC)]
                        def build_dg(c):
                            DG = DGs[c % 2]
                            k.op("dve", lambda e, c=c, DG=DG: e.tensor_tensor(
                                out=DG[:], in0=IDB[:].unsqueeze(1).broadcast_to([128, CK, 128]),
                                in1=PARL[:, c, P_WDW:P_WDW + CK].unsqueeze(2).broadcast_to([128, CK, 128]),
                                op=ALU.mult), reads=[IDB, PARL], writes=[DG])
                        build_dg(0)
                        build_dg(1)
                        for half in range(2):
                            sa = ws.get([slab_spec(win, half * 512, 512, NKC)])
                            sb_ = ws.get([slab_spec(win, D + half * 512, 512, NKC)])
                            wa_, wb_ = wview(sa, NKC, 512), wview(sb_, NKC, 512)
                            for j in range(4):
                                c = half * 4 + j
                                pa, pb = k.ps(), k.ps()
                                proj_fm(sa, wa_, j, H1, pa)
                                proj_fm(sb_, wb_, j, H1, pb)
                                sg = scratch()
                                k.op("act", lambda e, sg=sg, pb=pb: e.activation(out=sg[:], in_=pb[:], func=AF.Sigmoid),
                                     reads=[pb], writes=[sg])
                                k.op("dve", lambda e, c=c: e.tensor_copy(out=U[c][:, 0:CK - 1], in_=HALO[:, c, :]),
                                     reads=[HALO], writes=[U[c]])
                                k.op("dve", lambda e, c=c, sg=sg, pa=pa: e.tensor_tensor(out=U[c][:, CK - 1:], in0=pa[:], in1=sg[:],
                                                                                        op=ALU.mult),
                                     reads=[pa, sg, U[c]], writes=[U[c]])
                                k.op("dve", lambda e, c=c: e.tensor_copy(out=HALO[:, c, :], in_=U[c][:, GT:GT + CK - 1]),
                                     reads=[U[c]], writes=[HALO])
                            ws.release(2)
                        for c in range(NKC):
                            DG = DGs[c % 2]
                            if c >= 2:
                                build_dg(c)
                            pc = k.ps()
                            for tap in range(CK):
                                k.op("pe", lambda e, c=c, tap=tap, DG=DG, pc=pc: e.matmul(pc[:], lhsT=DG[:, tap, :], rhs=U[c][:, tap:tap + GT],
                                                                          start=(tap == 0), stop=(tap == CK - 1)),
                                     reads=[DG, U[c]], writes=[pc], inc=(tap == CK - 1))
                            k.op("act", lambda e, c=c, pc=pc: e.activation(out=CO[c][:], in_=pc[:], func=AF.Identity,
                                                                          bias=PARL[:, c, P_BDW:P_BDW + 1]),
                                 reads=[pc, PARL], writes=[CO[c]])
                        mu, rstd = ln_stats(scratch, [(CO[c], CO[c][:]) for c in range(NKC)], MU, RSTD, True)
                        for c in range(NKC):
                            y = scratch()
                            k.op("dve", lambda e, c=c, y=y: e.tensor_tensor(out=y[:], in0=CO[c][:], in1=mu[:], op=ALU.subtract),
                                 reads=[CO[c], mu], writes=[y])
                            k.op("dve", lambda e, y=y: e.tensor_tensor(out=y[:], in0=y[:], in1=rstd[:], op=ALU.mult),
                                 reads=[y, rstd], writes=[y])
                            k.op("act", lambda e, c=c, y=y: e.activation(out=CO[c][:], in_=y[:], func=AF.Silu,
                                                                        scale=PARL[:, c, P_LNCG:P_LNCG + 1],
                                                                        bias=PARL[:, c, P_LNCB:P_LNCB + 1]),
                                 reads=[y, PARL], writes=[CO[c]])
                        for half in range(2):
                            sgc = ws.get([slab_spec(win, 8192 + half * 512, 512, NKC)])
                            wg_ = wview(sgc, NKC, 512)
                            for j in range(4):
                                oc = half * 4 + j
                                pg = k.ps()
                                proj_fm(sgc, wg_, j, H1, pg)
                                k.op("act", lambda e, oc=oc, pg=pg: e.activation(out=SG[oc][:], in_=pg[:], func=AF.Sigmoid),
                                     reads=[pg], writes=[SG[oc]])
                            ws.release(1)
                        for half in range(2):
                            so = ws.get([slab_spec(w_conv_o[L], half * 512, 512, NKC)])
                            wo_ = wview(so, NKC, 512)
                            for j in range(4):
                                oc = half * 4 + j
                                py = k.ps()
                                proj_fm(so, wo_, j, CO, py)
                                k.op("dve", lambda e, oc=oc, py=py: e.tensor_tensor(out=M[oc][:], in0=py[:], in1=SG[oc][:], op=ALU.mult),
                                     reads=[py, SG[oc]], writes=[M[oc]])
                            ws.release(1)
                    k.barrier(("pe", "act", "dve", "sp"), bufs=prev_staging)
                    with ExitStack() as sp_:
                        VT = [[k.sb([128, DV], BF16, sp_) for _ in range(4)] for _ in range(2)]
                        KT = [[k.sb([128, DK], BF16, sp_) for _ in range(4)] for _ in range(2)]
                        RG = k.sb([128, 4, GT], BF16, sp_)
                        QD = [k.sb([128, 2, GT], BF16, sp_) for _ in range(2)]
                        PT = [k.sb([128, 4, 128], BF16, sp_) for _ in range(2)]
                        ON = [k.sb([128, DV], BF16, sp_) for _ in range(2)]
                        COS = k.sb([128, GT], F32, sp_)
                        SIN = k.sb([128, GT], F32, sp_)
                        SMALL = [k.sb([128, 64], F32, sp_) for _ in range(2)]
                        OG = [k.sb([128, 4, GT], BF16, sp_) for _ in range(HEADS)]
                        QK = [[k.sb([128, GT], BF16, sp_) for _ in range(4)] for _ in range(2)]
                        k.dma("sp", [(COS[:], cs_scr[0, :, cols])], reads=[CSB], writes=[COS])
                        k.dma("sp", [(SIN[:], cs_scr[1, :, cols])], reads=[CSB], writes=[SIN])
                        k.ps_pool = [0, 1, 2, 3]
                        PO = [k.psb[4 + i] for i in range(4)]

                        def A0(h):
                            qk = QK[h % 2]
                            sqk = ws.get([slab_spec(win, 2 * D + h * DK, DK, NKC, 0, 512),
                                          slab_spec(win, 3 * D + h * DK, DK, NKC, 256, 512)])
                            wqk = wview(sqk, NKC, 512)
                            for qi in range(2):
                                p1, p2 = k.ps(), k.ps()
                                proj_fm(sqk, wqk, qi * 2, H1, p1)
                                proj_fm(sqk, wqk, qi * 2 + 1, H1, p2)
                                t1, t2, t3, t4 = scratch(), scratch(), scratch(), scratch()
                                for (t, p, tab) in ((t1, p1, COS), (t2, p2, SIN), (t3, p2, COS), (t4, p1, SIN)):
                                    k.op("dve", lambda e, t=t, p=p, tab=tab: e.tensor_tensor(out=t[:], in0=p[:], in1=tab[:], op=ALU.mult),
                                         reads=[p, tab], writes=[t])
                                k.op("dve", lambda e, t1=t1, t2=t2, qi=qi: e.tensor_tensor(out=qk[qi * 2][:], in0=t1[:], in1=t2[:],
                                                                                          op=ALU.subtract),
                                     reads=[t1, t2], writes=[qk[qi * 2]])
                                k.op("dve", lambda e, t3=t3, t4=t4, qi=qi: e.tensor_tensor(out=qk[qi * 2 + 1][:], in0=t3[:], in1=t4[:],
                                                                                          op=ALU.add),
                                     reads=[t3, t4], writes=[qk[qi * 2 + 1]])
                            ws.release(1)

                        def A1(h):
                            vt = VT[h % 2]
                            sv = ws.get([slab_spec(win, 4 * D + h * DV, DV, NKC)])
                            wv_ = wview(sv, NKC, 512)
                            for tt in range(4):
                                pv = k.ps()
                                for kc in range(NKC):
                                    k.op("pe", lambda e, kc=kc, tt=tt, pv=pv: e.matmul(
                                        pv[:], lhsT=H1[kc][:, tt * 128:(tt + 1) * 128], rhs=wv_[:, kc, :],
                                        start=(kc == 0), stop=(kc == NKC - 1)),
                                        reads=[sv, H1[kc]], writes=[pv], inc=(kc == NKC - 1))
                                k.op("act", lambda e, tt=tt, pv=pv: e.activation(out=vt[tt][:], in_=pv[:], func=AF.Identity),
                                     reads=[pv], writes=[vt[tt]])
                            ws.release(1)

                        def A2(h):
                            srg = ws.get([slab_spec(win, 6 * D + h * DV, DV, NKC)])
                            wrg = wview(srg, NKC, 512)
                            for vc in range(4):
                                pr = k.ps()
                                proj_fm(srg, wrg, vc, H1, pr)
                                k.op("act", lambda e, vc=vc, pr=pr: e.activation(out=RG[:, vc, :], in_=pr[:], func=AF.Silu),
                                     reads=[pr], writes=[RG])
                            ws.release(1)

                        def A3(h):
                            qk, kt = QK[h % 2], KT[h % 2]
                            for tt in range(4):
                                pk = k.ps()
                                pkb = pk[:].bitcast(BF16)
                                for dc in range(2):
                                    k.op("pe", lambda e, tt=tt, dc=dc, pkb=pkb: e.transpose(
                                        out=pkb[:, dc * 128:(dc + 1) * 128], in_=qk[2 + dc][:, tt * 128:(tt + 1) * 128],
                                        identity=IDB[:]), reads=[qk[2 + dc], IDB], writes=[pk], inc=(dc == 1))
                                k.op("dve", lambda e, tt=tt, pkb=pkb, h=h: e.tensor_scalar(
                                    out=kt[tt][:], in0=pkb[:, 0:DK], scalar1=CON[:, C_KDEC + h:C_KDEC + h + 1], scalar2=None,
                                    op0=ALU.mult), reads=[pk, CON], writes=[kt[tt]])

                        def S123(h):
                            hb = h % 2
                            qk, vt, kt = QK[hb], VT[hb], KT[hb]
                            pt, qd, sm = PT[hb], QD[hb], SMALL[hb]
                            cdec = float(np.exp(np.float32(128.0) * np.log1p(np.float32(-2.0 ** (-5.0 - h)))))
                            mask = CON[:, C_MASK + h * 128:C_MASK + (h + 1) * 128]
                            qdec = CON[:, C_QDEC + h * 128:C_QDEC + (h + 1) * 128]
                            for dc in range(2):
                                k.op("act", lambda e, dc=dc: e.activation(out=STB[0][dc][:], in_=ST32[h][dc][:], func=AF.Identity),
                                     reads=[ST32[h][dc]], writes=[STB[0][dc]])
                            pss = k.ps()
                            for tt in range(4):
                                tc_ = slice(tt * 128, (tt + 1) * 128)
                                for dc in range(2):
                                    k.op("pe", lambda e, dc=dc, tc_=tc_: e.matmul(
                                        pss[:, tc_], lhsT=qk[2 + dc][:, tc_], rhs=qk[dc][:, tc_], start=(dc == 0), stop=(dc == 1)),
                                        reads=[qk[2 + dc], qk[dc]], writes=[pss], inc=(dc == 1 and tt == 3))
                            k.op("dve", lambda e: e.tensor_tensor(
                                out=pt[:], in0=pss[:].rearrange("p (t i) -> p t i", t=4),
                                in1=mask.unsqueeze(1).broadcast_to([128, 4, 128]), op=ALU.mult), reads=[pss, CON], writes=[pt])
                            for dc in range(2):
                                k.op("dve", lambda e, dc=dc: e.tensor_tensor(
                                    out=qd[:, dc, :].rearrange("p (t i) -> p t i", t=4), in0=qk[dc][:].rearrange("p (t i) -> p t i", t=4),
                                    in1=qdec.unsqueeze(1).broadcast_to([128, 4, 128]), op=ALU.mult), reads=[qk[dc], CON, qd], writes=[qd])
                            for tt in range(4):
                                tc_ = slice(tt * 128, (tt + 1) * 128)
                                sp_par, sn_par = tt % 2, (tt + 1) % 2
                                for dc in range(2):
                                    pd = k.ps()
                                    k.op("pe", lambda e, dc=dc, tt=tt, pd=pd: e.matmul(
                                        pd[:], lhsT=kt[tt][:, dc * 128:(dc + 1) * 128], rhs=vt[tt][:], start=True, stop=True),
                                        reads=[kt[tt], vt[tt]], writes=[pd])
                                    k.op("dve", lambda e, dc=dc, pd=pd: e.scalar_tensor_tensor(
                                        out=ST32[h][dc][:], in0=ST32[h][dc][:], scalar=cdec, in1=pd[:], op0=ALU.mult, op1=ALU.add),
                                        reads=[ST32[h][dc], pd], writes=[ST32[h][dc]])
                                    if tt < 3:
                                        k.op("act", lambda e, dc=dc, sn_par=sn_par: e.activation(out=STB[sn_par][dc][:], in_=ST32[h][dc][:],
                                                                                                func=AF.Identity),
                                             reads=[ST32[h][dc]], writes=[STB[sn_par][dc]])
                                po = PO[tt]
                                k.op("pe", lambda e, tt=tt, po=po: e.matmul(po[:], lhsT=pt[:, tt, :], rhs=vt[tt][:], start=True, stop=False),
                                     reads=[pt, vt[tt]], writes=[po], inc=False)
                                for dc in range(2):
                                    k.op("pe", lambda e, dc=dc, po=po, tc_=tc_, sp_par=sp_par: e.matmul(
                                        po[:], lhsT=qd[:, dc, tc_], rhs=STB[sp_par][dc][:], start=False, stop=(dc == 1)),
                                        reads=[qd, STB[sp_par][dc]], writes=[po], inc=(dc == 1))
                                k.op("dve", lambda e, tt=tt, po=po: e.bn_stats(out=sm[:, tt * 6:(tt + 1) * 6], in_=po[:]),
                                     reads=[po], writes=[sm])
                            for tt in range(4):
                                k.op("dve", lambda e, tt=tt: e.bn_aggr(out=sm[:, 24 + tt * 2:26 + tt * 2], in_=sm[:, tt * 6:(tt + 1) * 6]),
                                     reads=[sm], writes=[sm])
                            mv = sm[:, 24:32].rearrange("p (t two) -> p t two", two=2)
                            k.op("dve", lambda e: e.tensor_scalar(out=sm[:, 32:36], in0=mv[:, :, 1], scalar1=EPS, scalar2=None, op0=ALU.add),
                                 reads=[sm], writes=[sm])
                            k.op("act", lambda e: e.activation(out=sm[:, 36:40], in_=sm[:, 32:36], func=AF.Sqrt), reads=[sm], writes=[sm])
                            k.op("dve", lambda e: e.reciprocal(out=sm[:, 40:44], in_=sm[:, 36:40]), reads=[sm], writes=[sm])
                            k.op("dve", lambda e: e.scalar_tensor_tensor(out=sm[:, 44:48], in0=mv[:, :, 0], scalar=-1.0, in1=sm[:, 40:44],
                                                                          op0=ALU.mult, op1=ALU.mult), reads=[sm], writes=[sm])

                        def S4(h):
                            sm = SMALL[h % 2]

                            def evac(tt):
                                on, po = ON[tt % 2], PO[tt]
                                k.op("act", lambda e, on=on, po=po, tt=tt: e.activation(out=on[:], in_=po[:], func=AF.Identity,
                                                                                       scale=sm[:, 40 + tt:41 + tt], bias=sm[:, 44 + tt:45 + tt]),
                                     reads=[po, sm], writes=[on])
                            evac(0)
                            evac(1)
                            for tt in range(4):
                                tc_ = slice(tt * 128, (tt + 1) * 128)
                                on = ON[tt % 2]
                                ptb = k.ps()
                                ptbb = ptb[:].bitcast(BF16)
                                for vc in range(4):
                                    k.op("pe", lambda e, vc=vc, on=on, ptbb=ptbb: e.transpose(
                                        out=ptbb[:, vc * 128:(vc + 1) * 128], in_=on[:, vc * 128:(vc + 1) * 128], identity=IDB[:]),
                                        reads=[on, IDB], writes=[ptb], inc=(vc == 3))
                                if tt + 2 < 4:
                                    evac(tt + 2)
                                k.op("dve", lambda e, tc_=tc_, ptbb=ptbb: e.tensor_tensor(
                                    out=OG[h][:, :, tc_], in0=ptbb[:, 0:512].rearrange("p (v t) -> p v t", v=4), in1=RG[:, :, tc_],
                                    op=ALU.mult), reads=[ptb, RG], writes=[OG[h]])

                        A0(0); A1(0); A2(0); A3(0)
                        for h in range(HEADS):
                            S123(h)
                            if h + 1 < HEADS:
                                A0(h + 1)
                                A1(h + 1)
                            S4(h)
                            if h + 1 < HEADS:
                                A2(h + 1)
                                A3(h + 1)
                        for q4 in range(4):
                            sr = ws.get([slab_spec(w_ret_o[L], q4 * 256, 256, 16)])
                            wr_ = wview(sr, 16, 256)
                            if q4 % 2 == 0:
                                sgr = ws.get([slab_spec(win, 9 * D + (q4 // 2) * 512, 512, NKC)])
                                wgr = wview(sgr, NKC, 512)
                            for j in range(2):
                                oc = q4 * 2 + j
                                py, pg = k.ps(), k.ps()
                                for kc in range(16):
                                    k.op("pe", lambda e, kc=kc, j=j, py=py: e.matmul(
                                        py[:], lhsT=wr_[:, kc, j * 128:(j + 1) * 128], rhs=OG[kc // 4][:, kc % 4, :],
                                        start=(kc == 0), stop=(kc == 15)), reads=[sr, OG[kc // 4]], writes=[py], inc=(kc == 15))
                                proj_fm(sgr, wgr, (q4 % 2) * 2 + j, H1, pg)
                                sg, tm = scratch(), scratch()
                                k.op("act", lambda e, sg=sg, pg=pg: e.activation(out=sg[:], in_=pg[:], func=AF.Sigmoid),
                                     reads=[pg], writes=[sg])
                                k.op("dve", lambda e, sg=sg, py=py, tm=tm: e.tensor_tensor(out=tm[:], in0=py[:], in1=sg[:], op=ALU.mult),
                                     reads=[py, sg], writes=[tm])
                                k.op("dve", lambda e, oc=oc, tm=tm: e.tensor_tensor(out=M[oc][:], in0=M[oc][:], in1=tm[:], op=ALU.add),
                                     reads=[M[oc], tm], writes=[M[oc]])
                            ws.release(1)
                            if q4 % 2 == 1:
                                ws.release(1)
                        snap = k.snapshot()
                        if g + 1 < NG:
                            emit_h1(g + 1)
                        for half in range(2):
                            so = ws.get([slab_spec(w_out[L], half * 512, 512, NKC)])
                            wo_ = wview(so, NKC, 512)
                            for j in range(4):
                                oc = half * 4 + j
                                pm = k.ps()
                                proj_fm(so, wo_, j, M, pm)
                                k.op("dve", lambda e, oc=oc, pm=pm: e.scalar_tensor_tensor(
                                    out=XA[oc][:, cols], in0=pm[:], scalar=V[:, V_G1, oc:oc + 1], in1=XA[oc][:, cols],
                                    op0=ALU.mult, op1=ALU.add), reads=[pm, V, XA[oc]], writes=[XA[oc]])
                            ws.release(1)
                        mu, rstd = ln_stats(scratch, [(XA[c], XA[c][:, cols]) for c in range(NKC)], MU, RSTD, False)
                        for c in range(NKC):
                            y = scratch()
                            k.op("dve", lambda e, c=c, y=y: e.tensor_tensor(out=y[:], in0=XA[c][:, cols], in1=mu[:], op=ALU.subtract),
                                 reads=[XA[c], mu], writes=[y])
                            k.op("dve", lambda e, y=y: e.tensor_tensor(out=y[:], in0=y[:], in1=rstd[:], op=ALU.mult),
                                 reads=[y, rstd], writes=[y])
                            k.op("act", lambda e, c=c, y=y: e.activation(out=XA[c][:, cols], in_=y[:], func=AF.Identity,
                                                                        scale=V[:, V_XS1, c:c + 1], bias=V[:, V_XB1, c:c + 1]),
                                 reads=[y, V], writes=[XA[c]])
                            hh = QK[0][c % 4] if c < 4 else QK[1][c % 4]
                            k.op("act", lambda e, c=c, y=y, hh=hh: e.activation(out=hh[:], in_=y[:], func=AF.Identity,
                                                                               scale=V[:, V_GS2, c:c + 1], bias=V[:, V_BS2, c:c + 1]),
                                 reads=[y, V], writes=[hh])
                            k.dma("sp", [(h2_scr[c, :, cols], hh[:])], reads=[hh], writes=[H2B[c]])
                        if g == NG - 1:
                            k.barrier(("pe", "act", "dve", "sp", "pool"), bufs=QK[0] + QK[1])
                        else:
                            prev_snap, prev_staging = snap, QK[0] + QK[1]
                    k.ps_pool = list(range(8))

        def ffn(li, l):
            L = lw(l)
            V = VEC[li]
            moe = (l % 2 == 1)
            i2 = (l // 2) if fused else 0
            with ExitStack() as ph:
                H2 = [k.sb([128, T], BF16, ph) for _ in range(NKC)]
                A = [[k.sb([128, T], BF16, ph) for _ in range(FB)] for _ in range(2)]
                scr = [k.sb([128, GT], F32, ph) for _ in range(3)]
                MU = k.sb([128, GT], F32, ph)
                RSTD = k.sb([128, GT], F32, ph)
                scr_i = [0]

                def scratch():
                    b = scr[scr_i[0] % len(scr)]
                    scr_i[0] += 1
                    return b
                for c in range(NKC):
                    k.dma("sp", [(H2[c][:], h2_scr[c])], reads=[H2B[c]], writes=[H2[c]])
                if moe:
                    CMB = k.sb([128, 16, NE], F32, ph)
                    CB = [k.sb([128, T], BF16, ph) for _ in range(2)]
                    SM = [k.sb([128, 64], F32, ph) for _ in range(2)]
                    DGF = [k.sb([128, 128], F32, ph) for _ in range(2)]
                    for tt in range(16):
                        tc_ = slice(tt * 128, (tt + 1) * 128)
                        sm = SM[tt % 2]
                        pl = k.ps()
                        for kc in range(NKC):
                            k.op("pe", lambda e, kc=kc, tc_=tc_, pl=pl: e.matmul(pl[:, 0:NE], lhsT=H2[kc][:, tc_], rhs=WRB[:, i2, kc, :],
                                                                                start=(kc == 0), stop=(kc == NKC - 1)),
                                 reads=[H2[kc], WRB], writes=[pl], inc=(kc == NKC - 1))
                        lg, m1, mk1, l2, m2, mk2, w1, w2 = (sm[:, 0:8], sm[:, 8:9], sm[:, 16:24], sm[:, 24:32], sm[:, 9:10],
                                                            sm[:, 32:40], sm[:, 10:11], sm[:, 11:12])

                        def sop(fn, eng="dve", sm=sm, extra=()):
                            k.op(eng, fn, reads=[sm, *extra], writes=[sm])
                        k.op("dve", lambda e, lg=lg, pl=pl: e.tensor_copy(out=lg, in_=pl[:, 0:NE]), reads=[pl], writes=[sm])
                        sop(lambda e, lg=lg, m1=m1: e.tensor_reduce(out=m1, in_=lg, axis=mybir.AxisListType.X, op=ALU.max))
                        sop(lambda e, lg=lg, m1=m1, mk1=mk1: e.tensor_scalar(out=mk1, in0=lg, scalar1=m1, scalar2=None, op0=ALU.is_ge))
                        sop(lambda e, lg=lg, mk1=mk1, l2=l2: e.scalar_tensor_tensor(out=l2, in0=mk1, scalar=-1e30, in1=lg,
                                                                                   op0=ALU.mult, op1=ALU.add))
                        sop(lambda e, l2=l2, m2=m2: e.tensor_reduce(out=m2, in_=l2, axis=mybir.AxisListType.X, op=ALU.max))
                        sop(lambda e, l2=l2, m2=m2, mk2=mk2: e.tensor_scalar(out=mk2, in0=l2, scalar1=m2, scalar2=None, op0=ALU.is_ge))
                        sop(lambda e, m1=m1, m2=m2, w2=w2: e.tensor_tensor(out=w2, in0=m2, in1=m1, op=ALU.subtract))
                        sop(lambda e, w2=w2: e.activation(out=w2, in_=w2, func=AF.Sigmoid), eng="act")
                        sop(lambda e, w1=w1, w2=w2: e.tensor_scalar(out=w1, in0=w2, scalar1=-1.0, scalar2=1.0, op0=ALU.mult, op1=ALU.add))
                        sop(lambda e, mk1=mk1, w1=w1: e.tensor_scalar(out=mk1, in0=mk1, scalar1=w1, scalar2=None, op0=ALU.mult))
                        k.op("dve", lambda e, mk1=mk1, mk2=mk2, w2=w2, tt=tt: e.scalar_tensor_tensor(
                            out=CMB[:, tt, :], in0=mk2, scalar=w2, in1=mk1, op0=ALU.mult, op1=ALU.add), reads=[sm], writes=[CMB])

                nexp = NE if moe else 1
                blocks = [(b0, min(FB, NFC - b0)) for b0 in range(0, NFC, FB)]
                bi = 0
                ada_tasks = []
                if li + 1 < NL:
                    ln_ = layers[li + 1]
                    PARN = k.sb([128, NKC, NP_], F32, ph)
                    ASL = [k.sb([128, NKC, 512], BF16, ph) for _ in range(2)]
                    ADAN = k.sb([128, 48], F32, ph)
                    k.dma("sp", [(PARN[:], params.rearrange("p (l c n) -> p l c n", l=LW, c=NKC)[:, lw(ln_)])], writes=[PARN])
                    wan = w_ada[lw(ln_)].rearrange("(kc p) m -> p kc m", p=128)
                    pan = k.psb[7]
                    k.ps_pool = list(range(7))
                    k.barrier(("pool",))
                    k.dma("pool", [(ASL[0][:], wan[:, :, 0:512])], writes=[ASL[0]])

                    def ada_task(j):
                        if j + 1 < 12:
                            k.dma("pool", [(ASL[(j + 1) % 2][:], wan[:, :, (j + 1) * 512:(j + 2) * 512])], writes=[ASL[(j + 1) % 2]])
                        slab = ASL[j % 2]
                        for jj in range(4):
                            oc = j * 4 + jj
                            for kc in range(NKC):
                                k.op("pe", lambda e, jj=jj, kc=kc, oc=oc: e.matmul(
                                    pan[:, oc:oc + 1], lhsT=slab[:, kc, jj * 128:(jj + 1) * 128], rhs=CAB[:, kc:kc + 1],
                                    start=(kc == 0), stop=(kc == NKC - 1)), reads=[slab, CAB], writes=[pan], inc=(kc == NKC - 1))
                        if j == 11:
                            compute_vec(li + 1, ln_, pan, ADAN, PARN, lambda c: PARN[:, :, c])
                    ada_tasks = list(range(12))
                    per_block = 2 if not moe else 1
                for ex in range(nexp):
                    if moe:
                        wgm, wum, wdm = moe_wg[i2, ex], moe_wu[i2, ex], moe_wd[i2, ex]
                        cb = CB[ex % 2]
                        for tt in range(16):
                            dg = DGF[tt % 2]
                            k.op("dve", lambda e, dg=dg, tt=tt, ex=ex: e.tensor_scalar(out=dg[:], in0=ident_f, scalar1=CMB[:, tt, ex:ex + 1],
                                                                                      scalar2=None, op0=ALU.mult),
                                 reads=[CON, CMB], writes=[dg])
                            if tt % 4 == 0:
                                pcb = k.ps()
                            k.op("pe", lambda e, dg=dg, tt=tt, pcb=pcb: e.matmul(pcb[:, (tt % 4) * 128:(tt % 4 + 1) * 128], lhsT=ones_f,
                                                                                rhs=dg[:], start=True, stop=True),
                                 reads=[CON, dg], writes=[pcb])
                            if tt % 4 == 3:
                                k.op("act", lambda e, cb=cb, tt=tt, pcb=pcb: e.activation(out=cb[:, (tt // 4) * GT:(tt // 4 + 1) * GT], in_=pcb[:], func=AF.Identity),
                                     reads=[pcb], writes=[cb])
                    else:
                        wgm, wum, wdm = ffn_wg[i2], ffn_wu[i2], ffn_wd[i2]
                    for (b0, nb) in blocks:
                        Ab = A[bi % 2]
                        bi += 1
                        sg_ = ws.get([slab_spec(wgm, b0 * 128, nb * 128, NKC, 0, 512)])
                        su_ = ws.get([slab_spec(wum, b0 * 128, nb * 128, NKC, 0, 512)])
                        wdsrc = wdm[b0 * 128:(b0 + nb) * 128, :].rearrange("(f p) m -> p f m", p=128)
                        sd_ = ws.get([(lambda t, nb=nb: t[:, 0:nb * D].rearrange("p (f m) -> p f m", f=nb), wdsrc)])
                        wg_, wu_ = wview(sg_, NKC, 512), wview(su_, NKC, 512)
                        wd_ = sd_.t[:, 0:nb * D].rearrange("p (f m) -> p f m", f=nb)
                        for f in range(nb):
                            for g in range(NG):
                                cols = slice(g * GT, (g + 1) * GT)
                                pg, pu = k.ps(), k.ps()
                                for kc in range(NKC):
                                    k.op("pe", lambda e, kc=kc, f=f, pg=pg, cols=cols: e.matmul(
                                        pg[:], lhsT=wg_[:, kc, f * 128:(f + 1) * 128], rhs=H2[kc][:, cols],
                                        start=(kc == 0), stop=(kc == NKC - 1)), reads=[sg_, H2[kc]], writes=[pg], inc=(kc == NKC - 1))
                                for kc in range(NKC):
                                    k.op("pe", lambda e, kc=kc, f=f, pu=pu, cols=cols: e.matmul(
                                        pu[:], lhsT=wu_[:, kc, f * 128:(f + 1) * 128], rhs=H2[kc][:, cols],
                                        start=(kc == 0), stop=(kc == NKC - 1)), reads=[su_, H2[kc]], writes=[pu], inc=(kc == NKC - 1))
                                sgs = scratch()
                                k.op("act", lambda e, sgs=sgs, pg=pg: e.activation(out=sgs[:], in_=pg[:], func=AF.Silu),
                                     reads=[pg], writes=[sgs])
                                if moe:
                                    k.op("dve", lambda e, sgs=sgs, cb=cb, cols=cols: e.tensor_tensor(out=sgs[:], in0=sgs[:], in1=cb[:, cols],
                                                                                                    op=ALU.mult),
                                         reads=[sgs, cb], writes=[sgs])
                                k.op("dve", lambda e, sgs=sgs, pu=pu, f=f, cols=cols, Ab=Ab: e.tensor_tensor(
                                    out=Ab[f][:, cols], in0=pu[:], in1=sgs[:], op=ALU.mult), reads=[pu, sgs], writes=[Ab[f]])
                        ws.release(2)
                        for _ in range(per_block if ada_tasks else 0):
                            if ada_tasks:
                                ada_task(ada_tasks.pop(0))
                        for oc in range(NKC):
                            for g in range(NG):
                                cols = slice(g * GT, (g + 1) * GT)
                                pd = k.ps()
                                for f in range(nb):
                                    k.op("pe", lambda e, f=f, oc=oc, pd=pd, cols=cols, Ab=Ab: e.matmul(
                                        pd[:], lhsT=wd_[:, f, oc * 128:(oc + 1) * 128], rhs=Ab[f][:, cols],
                                        start=(f == 0), stop=(f == nb - 1)), reads=[sd_, Ab[f]], writes=[pd], inc=(f == nb - 1))
                                k.op("dve", lambda e, oc=oc, pd=pd, cols=cols: e.scalar_tensor_tensor(
                                    out=XA[oc][:, cols], in0=pd[:], scalar=V[:, V_G2, oc:oc + 1], in1=XA[oc][:, cols],
                                    op0=ALU.mult, op1=ALU.add), reads=[pd, V, XA[oc]], writes=[XA[oc]])
                        ws.release(1)
                assert not ada_tasks
                for g in range(NG):
                    cols = slice(g * GT, (g + 1) * GT)
                    mu, rstd = ln_stats(scratch, [(XA[c], XA[c][:, cols]) for c in range(NKC)], MU, RSTD, False)
                    for c in range(NKC):
                        k.op("dve", lambda e, c=c, cols=cols, mu=mu: e.tensor_tensor(out=XA[c][:, cols], in0=XA[c][:, cols], in1=mu[:],
                                                                                    op=ALU.subtract),
                             reads=[XA[c], mu], writes=[XA[c]])
                        k.op("dve", lambda e, c=c, cols=cols, rstd=rstd: e.tensor_tensor(out=XA[c][:, cols], in0=XA[c][:, cols], in1=rstd[:],
                                                                                        op=ALU.mult),
                             reads=[XA[c], rstd], writes=[XA[c]])
                        k.op("act", lambda e, c=c, cols=cols: e.activation(out=XA[c][:, cols], in_=XA[c][:, cols], func=AF.Identity,
                                                                          scale=V[:, V_XS2, c:c + 1], bias=V[:, V_XB2, c:c + 1]),
                             reads=[XA[c], V], writes=[XA[c]])

        k.dry = True
        emit()
        k.dry = False
        k.psi = 0
        ws.reset()
        emit()
    return nc


def _consts():
    c = np.zeros((128, C_END), np.float32)
    c[:, C_ID:C_ID + 128] = np.eye(128, dtype=np.float32)
    c[:, C_ONES:C_ONES + 128] = 1.0
    j = np.arange(128, dtype=np.float32)
    for h in range(HEADS):
        log_g = np.log1p(np.float32(-np.exp2(np.float32(-5.0 - h)))).astype(np.float32)
        diff = j[None, :] - j[:, None]
        m = np.where(diff >= 0, np.exp(log_g * np.maximum(diff, 0.0)), 0.0).astype(np.float32)
        c[:, C_MASK + h * 128:C_MASK + (h + 1) * 128] = m * np.float32(DK ** -0.5)
        c[:, C_QDEC + h * 128:C_QDEC + (h + 1) * 128] = np.exp(log_g * (j + 1.0)).astype(np.float32)[None, :]
        c[:, C_KDEC + h] = np.exp(log_g * (127.0 - j)).astype(np.float32) * np.float32(DK ** -0.5)
    half = 128
    c[:, C_INVF] = (10000.0 ** (-np.arange(half, dtype=np.float32) / half)).astype(np.float32)
    return c


def _params(inp, ls):
    out = np.zeros((128, len(ls), NKC, NP_), np.float32)
    for i, l in enumerate(ls):
        rows = [inp["b_dw"][l], inp["ln_conv_g"][l], inp["ln_conv_b"][l], inp["ln1_g"][l], inp["ln1_b"][l],
                inp["ln2_g"][l], inp["ln2_b"][l]] + [inp["w_dw"][l][t] for t in range(CK)] + \
               [inp["b_ada"][l].reshape(6, D)[j] for j in range(6)]
        m = np.stack(rows, 0).reshape(NP_, NKC, 128)
        out[:, i] = m.transpose(2, 1, 0)
    return np.ascontiguousarray(out.reshape(128, -1))


_PROG = {}


def _get_prog(key, layers, fused):
    if key not in _PROG:
        _PROG[key] = build_program(layers, fused)
    return _PROG[key]


FUSED = True


def kernel(**inp):
    inp = {k_: np.asarray(v) for k_, v in inp.items()}
    B = inp["x"].shape[0]
    consts = _consts()
    cores = list(range(B))
    xT = [np.ascontiguousarray(inp["x"][b].T) for b in range(B)]
    cTs = [np.ascontiguousarray(inp["c"][b].reshape(NKC, 128).T) for b in range(B)]
    posb = [np.ascontiguousarray(np.broadcast_to(inp["positions"][b].astype(np.int32)[None, :], (128, T))) for b in range(B)]

    def wr_layout(ws_):
        n = ws_.shape[0]
        return np.ascontiguousarray(ws_.reshape(n, NKC, 128, NE).transpose(2, 0, 1, 3).reshape(128, -1))

    if FUSED:
        nc = _get_prog("fused", list(range(DEPTH)), True)
        shared = dict(params=_params(inp, list(range(DEPTH))), consts=consts, w_ada=inp["w_ada"], w_in=inp["w_in"],
                      w_conv_o=inp["w_conv_o"], w_ret_o=inp["w_ret_o"], w_out=inp["w_out"],
                      ffn_w_gate=inp["ffn_w_gate"], ffn_w_up=inp["ffn_w_up"], ffn_w_down=inp["ffn_w_down"],
                      moe_wr=wr_layout(inp["moe_w_router"]), moe_w_gate=inp["moe_w_gate"], moe_w_up=inp["moe_w_up"],
                      moe_w_down=inp["moe_w_down"])
        in_maps = [dict(shared, xT=xT[b], cT=cTs[b], posb=posb[b]) for b in range(B)]
        res = run_bass_kernel_spmd(nc, in_maps, core_ids=cores)
        outs = [res.results[b]["outT"] for b in range(B)]
    else:
        cur = xT
        for l in range(DEPTH):
            moe = (l % 2 == 1)
            nc = _get_prog("moe" if moe else "dense", [l % 2], False)
            i2 = l // 2
            shared = dict(params=_params(inp, [l]), consts=consts, w_ada=inp["w_ada"][l:l + 1], w_in=inp["w_in"][l:l + 1],
                          w_conv_o=inp["w_conv_o"][l:l + 1], w_ret_o=inp["w_ret_o"][l:l + 1], w_out=inp["w_out"][l:l + 1])
            if moe:
                shared.update(moe_wr=wr_layout(inp["moe_w_router"][i2:i2 + 1]), moe_w_gate=inp["moe_w_gate"][i2:i2 + 1],
                              moe_w_up=inp["moe_w_up"][i2:i2 + 1], moe_w_down=inp["moe_w_down"][i2:i2 + 1])
            else:
                shared.update(ffn_w_gate=inp["ffn_w_gate"][i2:i2 + 1], ffn_w_up=inp["ffn_w_up"][i2:i2 + 1],
                              ffn_w_down=inp["ffn_w_down"][i2:i2 + 1])
            in_maps = [dict(shared, xT=cur[b], cT=cTs[b], posb=posb[b]) for b in range(B)]
            res = run_bass_kernel_spmd(nc, in_maps, core_ids=cores)
            cur = [res.results[b]["outT"] for b in range(B)]
        outs = cur
    return np.stack([o.T for o in outs], 0).astype(np.float32)
```

```python
import numpy as np
from contextlib import ExitStack
import concourse.bass as bass
import concourse.mybir as mybir
from concourse.bass_utils import run_bass_kernel_spmd

F32 = mybir.dt.float32
BF16 = mybir.dt.bfloat16
I32 = mybir.dt.int32
AF = mybir.ActivationFunctionType
ALU = mybir.AluOpType

DEPTH = 4
D = 1024
T = 2048
NKC = 8
GT = 512
NG = T // GT
DFF = 2816
NFC = DFF // 128
NE = 8
HEADS = 4
DK = 256
DV = 512
CK = 31
IN_W = 10240
ALPHA = float((2 * DEPTH) ** 0.25)
EPS = 1e-5
P_BDW, P_LNCG, P_LNCB, P_LN1G, P_LN1B, P_LN2G, P_LN2B, P_WDW, P_BADA = 0, 1, 2, 3, 4, 5, 6, 7, 38
NP_ = 44
V_S1, V_SH1, V_G1, V_XS1, V_XB1, V_GS2, V_BS2, V_G2, V_XS2, V_XB2, V_TMP = range(11)
NV = 11
C_ID, C_ONES, C_MASK, C_QDEC, C_KDEC, C_INVF, C_END = 0, 128, 256, 768, 1280, 1284, 1285
NRING = 4
FB = 4


class Eng:
    def __init__(self, name, e, sem):
        self.name, self.e, self.sem, self.n, self.known = name, e, sem, 0, {}


class Buf:
    def __init__(self, t):
        self.t = t
        self.w = None
        self.r = {}
        self.dsem = None
        self.dn = 0

    def __getitem__(self, idx):
        return self.t[idx]


class Ctx:
    def __init__(self, nc, es):
        self.nc, self.es, self.dry = nc, es, False
        self.sems = {}
        self.eng = {}
        for name, e in [("pe", nc.tensor), ("act", nc.scalar), ("dve", nc.vector),
                        ("pool", nc.gpsimd), ("sp", nc.sync)]:
            sem = es.enter_context(nc.semaphore("s_" + name))
            self.sems[sem.num] = sem
            self.eng[name] = Eng(name, e, sem)
        self.uid = 0
        self.dpool = []
        for i in range(44):
            sem = es.enter_context(nc.semaphore(f"d{i}"))
            self.sems[sem.num] = sem
            self.dpool.append(sem)
        self.dpool_sw = []
        for i in range(8):
            sem = es.enter_context(nc.semaphore(f"w{i}"))
            self.sems[sem.num] = sem
            self.dpool_sw.append(sem)
        self.dcount = {}
        self.psb = [Buf(es.enter_context(nc.psum_tensor(f"psb{i}", [128, 512], F32))) for i in range(8)]
        self.psi = 0
        self.ps_pool = list(range(8))

    def sb(self, shape, dt, st=None, name=None):
        self.uid += 1
        st = st or self.es
        b = Buf(st.enter_context(self.nc.sbuf_tensor(f"{name or 'b'}{self.uid}", shape, dt)))
        b.st = st
        return b

    def dram(self, name, shape, dt, kind=None):
        if kind:
            return Buf(self.nc.dram_tensor(name, shape, dt, kind=kind))
        return Buf(self.nc.dram_tensor(name, shape, dt))

    def ps(self):
        pool = self.ps_pool
        b = self.psb[pool[self.psi % len(pool)]]
        self.psi += 1
        return b

    def _need(self, reads, writes):
        need = {}

        def add(k, v):
            if need.get(k, 0) < v:
                need[k] = v
        for b in reads:
            if b.w:
                add(*b.w)
        for b in writes:
            if b.w:
                add(*b.w)
            for k, v in b.r.items():
                add(k, v)
        return need

    def _wait(self, E, need):
        for k, v in need.items():
            if k == E.sem.num and E.name == "pe":
                continue
            if E.known.get(k, 0) >= v:
                continue
            E.e.wait_ge(self.sems[k], v)
            E.known[k] = v

    def op(self, eng, fn, reads=(), writes=(), inc=True):
        if self.dry:
            return None
        E = self.eng[eng]
        self._wait(E, self._need(reads, writes))
        ins = fn(E.e)
        if inc:
            E.n += 1
            ins.then_inc(E.sem, 1)
            tick = (E.sem.num, E.n)
        else:
            tick = (E.sem.num, E.n + 1)
        for b in reads:
            if b.r.get(tick[0], 0) < tick[1]:
                b.r[tick[0]] = tick[1]
        for b in writes:
            b.w = tick
            b.r = {}
        return ins

    def dma(self, eng, pairs, reads=(), writes=(), **kw):
        if self.dry:
            return
        E = self.eng[eng]
        self._wait(E, self._need(reads, writes))
        anchor = writes[0] if writes else reads[0]
        if anchor.dsem is None:
            pool = self.dpool_sw if eng == "pool" else self.dpool
            anchor.dsem = pool.pop(0)
            st = getattr(anchor, "st", None)
            if st is not None and st is not self.es:
                st.callback(lambda sem=anchor.dsem, pool=pool: pool.append(sem))
        num = anchor.dsem.num
        for o, i in pairs:
            E.e.dma_start(out=o, in_=i, **kw).then_inc(anchor.dsem, 16)
            self.dcount[num] = self.dcount.get(num, 0) + 16
        tick = (num, self.dcount[num])
        for b in reads:
            if b.r.get(tick[0], 0) < tick[1]:
                b.r[tick[0]] = tick[1]
        for b in writes:
            b.w = tick
            b.r = {}

    def barrier(self, engs=("pe", "act", "dve", "sp"), bufs=()):
        if self.dry:
            return
        for a in engs:
            A = self.eng[a]
            self._wait(A, self._need((), bufs))
            for b in ("pe", "act", "dve"):
                if a == b:
                    continue
                B = self.eng[b]
                if B.n > A.known.get(B.sem.num, 0):
                    A.e.wait_ge(B.sem, B.n)
                    A.known[B.sem.num] = B.n

    def snapshot(self):
        return {n: self.eng[n].n for n in ("pe", "act", "dve")}

    def barrier_to(self, engs, snap):
        if self.dry:
            return
        for a in engs:
            A = self.eng[a]
            for b, v in snap.items():
                if a == b:
                    continue
                B = self.eng[b]
                if v > A.known.get(B.sem.num, 0):
                    A.e.wait_ge(B.sem, v)
                    A.known[B.sem.num] = v

    def wait_buf(self, eng, b):
        if self.dry:
            return
        E = self.eng[eng]
        need = {}
        if b.w:
            need[b.w[0]] = b.w[1]
        self._wait(E, need)


class WStream:
    def __init__(self, k, n):
        self.k, self.n = k, n
        self.slabs = [k.sb([128, 4096], BF16, name="wr") for _ in range(n)]
        self.reqs = []
        self.i = self.loaded = self.released = 0

    def get(self, spec):
        if self.k.dry:
            self.reqs.append(spec)
            return self.slabs[0]
        idx = self.i
        self.i += 1
        self._pump()
        assert self.loaded > idx, "weight ring too small for holding pattern"
        return self.slabs[idx % self.n]

    def release(self, cnt=1):
        if self.k.dry:
            return
        self.released += cnt
        self._pump()

    def _pump(self):
        while self.loaded < len(self.reqs) and self.loaded < self.released + self.n:
            j = self.loaded
            slab = self.slabs[j % self.n]
            pairs = [(fn(slab.t), src) for fn, src in self.reqs[j]]
            self.k.dma("pool", pairs, writes=[slab])
            self.loaded += 1

    def reset(self):
        self.i = self.loaded = self.released = 0


def build_program(layers, fused):
    nc = bass.Bass("TRN2", target_bir_lowering=False)
    NL = len(layers)

    def din(name, shape, dt=F32):
        return nc.dram_tensor(name, shape, dt, kind="ExternalInput").ap()

    LW = DEPTH if fused else 1
    xT = din("xT", [D, T])
    cT = din("cT", [128, NKC])
    posb = din("posb", [128, T], I32)
    params = din("params", [128, LW * NKC * NP_])
    consts = din("consts", [128, C_END])
    w_ada = din("w_ada", [LW, D, 6 * D])
    w_in = din("w_in", [LW, D, IN_W])
    w_conv_o = din("w_conv_o", [LW, D, D])
    w_ret_o = din("w_ret_o", [LW, 2 * D, D])
    w_out = din("w_out", [LW, D, D])
    has_dense = any(l % 2 == 0 for l in layers)
    has_moe = any(l % 2 == 1 for l in layers)
    ND = 2 if fused else 1
    if has_dense:
        ffn_wg = din("ffn_w_gate", [ND, D, DFF])
        ffn_wu = din("ffn_w_up", [ND, D, DFF])
        ffn_wd = din("ffn_w_down", [ND, DFF, D])
    if has_moe:
        moe_wr = din("moe_wr", [128, ND * NKC * NE])
        moe_wg = din("moe_w_gate", [ND, NE, D, DFF])
        moe_wu = din("moe_w_up", [ND, NE, D, DFF])
        moe_wd = din("moe_w_down", [ND, NE, DFF, D])
    outT = nc.dram_tensor("outT", [D, T], F32, kind="ExternalOutput").ap()
    cs_scr = nc.dram_tensor("cs_scr", [2, 128, T], F32).ap()
    h2_scr = nc.dram_tensor("h2_scr", [NKC, 128, T], BF16).ap()

    with ExitStack() as es:
        k = Ctx(nc, es)
        OUTB = Buf(None)
        CSB = Buf(None)
        H2B = [Buf(None) for _ in range(NKC)]
        XA = [k.sb([128, T], F32, name="xa") for _ in range(NKC)]
        PARL = k.sb([128, NKC, NP_], F32, name="parl")
        CON = k.sb([128, C_END], F32, name="con")
        IDB = k.sb([128, 128], BF16, name="idb")
        ONB = k.sb([128, 128], BF16, name="onb")
        VEC = [k.sb([128, NV, NKC], F32, name="vec") for _ in range(NL)]
        CAB = k.sb([128, NKC], BF16, name="cab")
        ws = WStream(k, NRING)
        if has_moe:
            WRB = k.sb([128, ND, NKC, NE], BF16, name="wrb")

        ident_f = CON[:, C_ID:C_ID + 128]
        ones_f = CON[:, C_ONES:C_ONES + 128]

        def lw(l):
            return l if fused else 0

        def emit():
            k.dma("sp", [(CON[:], consts)], writes=[CON])
            for c in range(NKC):
                k.dma("sp", [(XA[c][:], xT[c * 128:(c + 1) * 128, :])], writes=[XA[c]])
            k.op("dve", lambda e: e.tensor_copy(out=IDB[:], in_=ident_f), reads=[CON], writes=[IDB])
            k.op("dve", lambda e: e.tensor_copy(out=ONB[:], in_=ones_f), reads=[CON], writes=[ONB])
            with ExitStack() as ph:
                PAR = k.sb([128, LW, NKC, NP_], F32, ph)
                k.dma("sp", [(PAR[:].rearrange("p l c n -> p (l c n)"), params)], writes=[PAR])
                if has_moe:
                    WRT = k.sb([128, ND, NKC, NE], F32, ph)
                    k.dma("sp", [(WRT[:].rearrange("p l c n -> p (l c n)"), moe_wr)], writes=[WRT])
                    k.op("dve", lambda e: e.tensor_copy(out=WRB[:], in_=WRT[:]), reads=[WRT], writes=[WRB])
                CT = k.sb([128, NKC], F32, ph)
                CA = k.sb([128, NKC], F32, ph)
                ADA = k.sb([128, 48], F32, ph)
                WA = [k.sb([128, NKC, 512], BF16, ph) for _ in range(3)]
                k.dma("sp", [(CT[:], cT)], writes=[CT])
                k.op("act", lambda e: e.activation(out=CA[:], in_=CT[:], func=AF.Silu), reads=[CT], writes=[CA])
                POSI = k.sb([128, T], I32, ph)
                k.dma("sp", [(POSI[:], posb)], writes=[POSI])
                sc = [k.sb([128, GT], F32, ph) for _ in range(4)]
                sci = k.sb([128, GT], I32, ph)
                invf = CON[:, C_INVF:C_INVF + 1]
                for g in range(NG):
                    cols = slice(g * GT, (g + 1) * GT)
                    ang, t_, tf, r_ = sc
                    k.op("dve", lambda e: e.tensor_copy(out=ang[:], in_=POSI[:, cols]), reads=[POSI], writes=[ang])
                    k.op("dve", lambda e: e.tensor_scalar(out=ang[:], in0=ang[:], scalar1=invf, scalar2=None, op0=ALU.mult),
                         reads=[ang, CON], writes=[ang])
                    for which, off in ((0, 0.75), (1, 0.5)):
                        k.op("dve", lambda e, off=off: e.tensor_scalar(out=t_[:], in0=ang[:], scalar1=float(1.0 / (2 * np.pi)),
                                                                       scalar2=off, op0=ALU.mult, op1=ALU.add),
                             reads=[ang], writes=[t_])
                        k.op("dve", lambda e: e.tensor_copy(out=sci[:], in_=t_[:]), reads=[t_], writes=[sci])
                        k.op("dve", lambda e: e.tensor_copy(out=tf[:], in_=sci[:]), reads=[sci], writes=[tf])
                        k.op("dve", lambda e: e.tensor_tensor(out=t_[:], in0=t_[:], in1=tf[:], op=ALU.subtract),
                             reads=[t_, tf], writes=[t_])
                        k.op("dve", lambda e: e.tensor_scalar(out=tf[:], in0=t_[:], scalar1=0.0, scalar2=None, op0=ALU.is_lt),
                             reads=[t_], writes=[tf])
                        k.op("dve", lambda e: e.tensor_tensor(out=t_[:], in0=t_[:], in1=tf[:], op=ALU.add),
                             reads=[t_, tf], writes=[t_])
                        k.op("dve", lambda e: e.tensor_scalar(out=tf[:], in0=t_[:], scalar1=1.0, scalar2=None, op0=ALU.is_ge),
                             reads=[t_], writes=[tf])
                        k.op("dve", lambda e: e.tensor_tensor(out=t_[:], in0=t_[:], in1=tf[:], op=ALU.subtract),
                             reads=[t_, tf], writes=[t_])
                        k.op("dve", lambda e: e.tensor_scalar(out=r_[:], in0=t_[:], scalar1=6.283185, scalar2=-3.1415925,
                                                              op0=ALU.mult, op1=ALU.add), reads=[t_], writes=[r_])
                        k.op("act", lambda e: e.activation(out=r_[:], in_=r_[:], func=AF.Sin), reads=[r_], writes=[r_])
                        k.dma("sp", [(cs_scr[which, :, cols], r_[:])], reads=[r_], writes=[CSB])
                si = 0
                k.op("dve", lambda e: e.tensor_copy(out=CAB[:], in_=CA[:]), reads=[CA], writes=[CAB])
                for li, l in enumerate(layers[:1]):
                    wa = w_ada[lw(l)].rearrange("(kc p) m -> p kc m", p=128)
                    pa = k.ps()
                    for s in range(12):
                        slab = WA[si % 3]
                        si += 1
                        k.dma("pool", [(slab[:], wa[:, :, s * 512:(s + 1) * 512])], writes=[slab])
                        for j in range(4):
                            oc = s * 4 + j
                            for kc in range(NKC):
                                k.op("pe", lambda e, slab=slab, j=j, kc=kc, oc=oc: e.matmul(
                                    pa[:, oc:oc + 1], lhsT=slab[:, kc, j * 128:(j + 1) * 128], rhs=CAB[:, kc:kc + 1],
                                    start=(kc == 0), stop=(kc == NKC - 1)),
                                    reads=[slab, CAB], writes=[pa], inc=(kc == NKC - 1))
                    compute_vec(li, l, pa, ADA, PAR, lambda c, l=l: PAR[:, lw(l), :, c])
                for c in range(NKC):
                    k.op("act", lambda e, c=c: e.activation(out=XA[c][:], in_=XA[c][:], func=AF.Identity, scale=ALPHA), reads=[XA[c]], writes=[XA[c]])
                k.barrier(bufs=sc)


            import os
            stage = os.environ.get("MK_STAGE", "all")
            for li, l in enumerate(layers):
                if stage in ("all", "mixer"):
                    mixer(li, l)
                    k.barrier()
                if stage in ("all", "ffn"):
                    ffn(li, l)
                    k.barrier()
                    k.ps_pool = list(range(8))

            for c in range(NKC):
                k.dma("sp", [(outT[c * 128:(c + 1) * 128, :], XA[c][:])], reads=[XA[c]], writes=[OUTB])
            k.wait_buf("sp", OUTB)

        def compute_vec(li, l, pa, ADA, PARB, Pp):
            for j in range(6):
                k.op("dve", lambda e, j=j: e.tensor_tensor(
                    out=ADA[:, j * 8:(j + 1) * 8], in0=pa[:, j * 8:(j + 1) * 8],
                    in1=Pp(P_BADA + j), op=ALU.add), reads=[pa, PARB], writes=[ADA])
            V = VEC[li]
            last = (fused and l == DEPTH - 1)

            def vop(fn):
                k.op("dve", fn, reads=[ADA, PARB, V], writes=[V])
            A_ = lambda j: ADA[:, j * 8:(j + 1) * 8]
            vop(lambda e: e.tensor_scalar(out=V[:, V_S1, :], in0=A_(1), scalar1=1.0, scalar2=1.0 / ALPHA, op0=ALU.add, op1=ALU.mult))
            vop(lambda e: e.tensor_copy(out=V[:, V_SH1, :], in_=A_(0)))
            vop(lambda e: e.tensor_copy(out=V[:, V_G1, :], in_=A_(2)))
            vop(lambda e: e.tensor_scalar(out=V[:, V_XS1, :], in0=Pp(P_LN1G), scalar1=ALPHA, scalar2=None, op0=ALU.mult))
            vop(lambda e: e.tensor_scalar(out=V[:, V_XB1, :], in0=Pp(P_LN1B), scalar1=ALPHA, scalar2=None, op0=ALU.mult))
            vop(lambda e: e.tensor_scalar(out=V[:, V_TMP, :], in0=A_(4), scalar1=1.0, scalar2=None, op0=ALU.add))
            vop(lambda e: e.tensor_tensor(out=V[:, V_GS2, :], in0=Pp(P_LN1G), in1=V[:, V_TMP, :], op=ALU.mult))
            vop(lambda e: e.tensor_tensor(out=V[:, V_BS2, :], in0=Pp(P_LN1B), in1=V[:, V_TMP, :], op=ALU.mult))
            vop(lambda e: e.tensor_tensor(out=V[:, V_BS2, :], in0=V[:, V_BS2, :], in1=A_(3), op=ALU.add))
            vop(lambda e: e.tensor_copy(out=V[:, V_G2, :], in_=A_(5)))
            a2 = 1.0 if (last or not fused) else ALPHA
            vop(lambda e: e.tensor_scalar(out=V[:, V_XS2, :], in0=Pp(P_LN2G), scalar1=a2, scalar2=None, op0=ALU.mult))
            vop(lambda e: e.tensor_scalar(out=V[:, V_XB2, :], in0=Pp(P_LN2B), scalar1=a2, scalar2=None, op0=ALU.mult))

        def ln_stats(ph_scr, srcs, mu, rstd, bf_src):
            p1 = k.ps()
            n = len(srcs)
            for i, (b, ap) in enumerate(srcs):
                if bf_src:
                    k.op("pe", lambda e, ap=ap, i=i: e.matmul(p1[:], lhsT=ONB[:], rhs=ap, start=(i == 0), stop=(i == n - 1)),
                         reads=[b, ONB], writes=[p1])
                else:
                    xb = ph_scr()
                    xbv = xb[:].bitcast(BF16)[:, 0:GT]
                    if i % 2 == 0:
                        k.op("dve", lambda e, ap=ap, xbv=xbv: e.tensor_copy(out=xbv, in_=ap), reads=[b], writes=[xb])
                    else:
                        k.op("act", lambda e, ap=ap, xbv=xbv: e.activation(out=xbv, in_=ap, func=AF.Identity), reads=[b], writes=[xb])
                    k.op("pe", lambda e, xbv=xbv, i=i: e.matmul(p1[:], lhsT=ONB[:], rhs=xbv, start=(i == 0), stop=(i == n - 1)),
                         reads=[xb, ONB], writes=[p1])
            p2 = k.ps()
            for i, (b, ap) in enumerate(srcs):
                sq = ph_scr()
                sqv = sq[:].bitcast(BF16)[:, 0:GT]
                k.op("act", lambda e, ap=ap, sqv=sqv: e.activation(out=sqv, in_=ap, func=AF.Square), reads=[b], writes=[sq])
                k.op("pe", lambda e, sqv=sqv, i=i: e.matmul(p2[:], lhsT=ONB[:], rhs=sqv, start=(i == 0), stop=(i == n - 1)),
                     reads=[sq, ONB], writes=[p2])
            return ln_chain(p1, p2, n, mu, rstd)

        def ln_chain(p1, p2, n, mu, rstd):
            dn = float(1.0 / (128 * n))
            k.op("dve", lambda e: e.tensor_scalar(out=mu[:], in0=p1[:], scalar1=dn, scalar2=None, op0=ALU.mult),
                 reads=[p1], writes=[mu])
            k.op("dve", lambda e: e.tensor_tensor(out=rstd[:], in0=mu[:], in1=mu[:], op=ALU.mult), reads=[mu], writes=[rstd])
            k.op("dve", lambda e: e.scalar_tensor_tensor(out=rstd[:], in0=p2[:], scalar=dn, in1=rstd[:], op0=ALU.mult,
                                                          op1=ALU.subtract), reads=[p2, rstd], writes=[rstd])
            k.op("dve", lambda e: e.tensor_scalar(out=rstd[:], in0=rstd[:], scalar1=EPS, scalar2=None, op0=ALU.add),
                 reads=[rstd], writes=[rstd])
            k.op("act", lambda e: e.activation(out=rstd[:], in_=rstd[:], func=AF.Sqrt), reads=[rstd], writes=[rstd])
            k.op("dve", lambda e: e.reciprocal(out=rstd[:], in_=rstd[:]), reads=[rstd], writes=[rstd])
            return mu, rstd

        def slab_spec(wmat, c0, ncols, nk, dst_off=0, tot=None):
            tot = tot or ncols
            src = wmat.rearrange("(kc p) m -> p kc m", p=128)[:, :, c0:c0 + ncols]
            return (lambda t, nk=nk, tot=tot, dst_off=dst_off, ncols=ncols:
                    t[:, 0:nk * tot].rearrange("p (kc m) -> p kc m", kc=nk)[:, :, dst_off:dst_off + ncols], src)

        def wview(slab, nk, tot):
            return slab.t[:, 0:nk * tot].rearrange("p (kc m) -> p kc m", kc=nk)

        def proj_fm(slab, wv, j, H1, pb):
            for kc in range(NKC):
                k.op("pe", lambda e, kc=kc: e.matmul(pb[:], lhsT=wv[:, kc, j * 128:(j + 1) * 128], rhs=H1[kc][:],
                                                     start=(kc == 0), stop=(kc == NKC - 1)),
                     reads=[slab, H1[kc]], writes=[pb], inc=(kc == NKC - 1))

        def mixer(li, l):
            L = lw(l)
            V = VEC[li]
            win = w_in[L]
            k.dma("sp", [(PARL[:], params.rearrange("p (l c n) -> p l c n", l=LW, c=NKC)[:, L])], writes=[PARL])
            with ExitStack() as ph:
                H1 = [k.sb([128, GT], BF16, ph) for _ in range(NKC)]
                ST32 = [[k.sb([128, DV], F32, ph) for _ in range(2)] for _ in range(HEADS)]
                STB = [[k.sb([128, DV], BF16, ph) for _ in range(2)] for _ in range(2)]
                HALO = k.sb([128, NKC, CK - 1], BF16, ph)
                M = [k.sb([128, GT], BF16, ph) for _ in range(NKC)]
                scr = [k.sb([128, GT], F32, ph) for _ in range(4)]
                MU = k.sb([128, GT], F32, ph)
                RSTD = k.sb([128, GT], F32, ph)
                scr_i = [0]

                def scratch():
                    b = scr[scr_i[0] % len(scr)]
                    scr_i[0] += 1
                    return b
                for h in range(HEADS):
                    for dc in range(2):
                        k.op("dve", lambda e, h=h, dc=dc: e.memset(ST32[h][dc][:], 0.0), writes=[ST32[h][dc]])
                k.op("dve", lambda e: e.memset(HALO[:], 0.0), writes=[HALO])

                prev_snap, prev_staging = None, []
                for g in range(NG):
                    cols = slice(g * GT, (g + 1) * GT)
                    k.ps_pool = list(range(8))
                    if prev_snap is not None:
                        k.barrier_to(("pe", "act", "dve", "sp"), prev_snap)
                    def emit_h1(gg):
                        cc = slice(gg * GT, (gg + 1) * GT)
                        for kc in range(NKC):
                            k.op("act", lambda e, kc=kc, cc=cc: e.activation(out=H1[kc][:], in_=XA[kc][:, cc], func=AF.Identity,
                                                                            scale=V[:, V_S1, kc:kc + 1], bias=V[:, V_SH1, kc:kc + 1]),
                                 reads=[XA[kc], V], writes=[H1[kc]])
                    if g == 0:
                        emit_h1(0)
                    with ExitStack() as sp_:
                        U = [k.sb([128, CK - 1 + GT], BF16, sp_) for _ in range(NKC)]
                        CO = [k.sb([128, GT], BF16, sp_) for _ in range(NKC)]
                        DGs = [k.sb([128, CK, 128], BF16, sp_) for _ in range(2)]
                        SG = [k.sb([128, GT], BF16, sp_) for _ in range(NKC)]
                        def build_dg(c):
                            DG = DGs[c % 2]
                            k.op("dve", lambda e, c=c, DG=DG: e.tensor_tensor(
                                out=DG[:], in0=IDB[:].unsqueeze(1).broadcast_to([128, CK, 128]),
                                in1=PARL[:, c, P_WDW:P_WDW + CK].unsqueeze(2).broadcast_to([128, CK, 128]),
                                op=ALU.mult), reads=[IDB, PARL], writes=[DG])
                        build_dg(0)
                        build_dg(1)
                        for half in range(2):
                            sa = ws.get([slab_spec(win, half * 512, 512, NKC)])
                            sb_ = ws.get([slab_spec(win, D + half * 512, 512, NKC)])
                            wa_, wb_ = wview(sa, NKC, 512), wview(sb_, NKC, 512)
                            for j in range(4):
                                c = half * 4 + j
                                pa, pb = k.ps(), k.ps()
                                proj_fm(sa, wa_, j, H1, pa)
                                proj_fm(sb_, wb_, j, H1, pb)
                                sg = scratch()
                                k.op("act", lambda e, sg=sg, pb=pb: e.activation(out=sg[:], in_=pb[:], func=AF.Sigmoid),
                                     reads=[pb], writes=[sg])
                                k.op("dve", lambda e, c=c: e.tensor_copy(out=U[c][:, 0:CK - 1], in_=HALO[:, c, :]),
                                     reads=[HALO], writes=[U[c]])
                                k.op("dve", lambda e, c=c, sg=sg, pa=pa: e.tensor_tensor(out=U[c][:, CK - 1:], in0=pa[:], in1=sg[:],
                                                                                        op=ALU.mult),
                                     reads=[pa, sg, U[c]], writes=[U[c]])
                                k.op("dve", lambda e, c=c: e.tensor_copy(out=HALO[:, c, :], in_=U[c][:, GT:GT + CK - 1]),
                                     reads=[U[c]], writes=[HALO])
                            ws.release(2)
                        k.ps_pool = list(range(6))
                        p1c, p2c = k.psb[6], k.psb[7]
                        sqs = {}

                        def conv_stats(c):
                            sqv = sqs[c][:].bitcast(BF16)[:, 0:GT]
                            k.op("pe", lambda e, c=c: e.matmul(p1c[:], lhsT=ONB[:], rhs=CO[c][:], start=(c == 0), stop=(c == NKC - 1)),
                                 reads=[CO[c], ONB], writes=[p1c])
                            k.op("pe", lambda e, c=c, sqv=sqv: e.matmul(p2c[:], lhsT=ONB[:], rhs=sqv, start=(c == 0), stop=(c == NKC - 1)),
                                 reads=[sqs[c], ONB], writes=[p2c])
                        for c in range(NKC):
                            DG = DGs[c % 2]
                            if c >= 2:
                                build_dg(c)
                            pc = k.ps()
                            for tap in range(CK):
                                k.op("pe", lambda e, c=c, tap=tap, DG=DG, pc=pc: e.matmul(pc[:], lhsT=DG[:, tap, :], rhs=U[c][:, tap:tap + GT],
                                                                          start=(tap == 0), stop=(tap == CK - 1)),
                                     reads=[DG, U[c]], writes=[pc], inc=(tap == CK - 1))
                            k.op("act", lambda e, c=c, pc=pc: e.activation(out=CO[c][:], in_=pc[:], func=AF.Identity,
                                                                          bias=PARL[:, c, P_BDW:P_BDW + 1]),
                                 reads=[pc, PARL], writes=[CO[c]])
                            sqs[c] = scratch()
                            k.op("act", lambda e, c=c, sqv=sqs[c][:].bitcast(BF16)[:, 0:GT]: e.activation(out=sqv, in_=CO[c][:], func=AF.Square),
                                 reads=[CO[c]], writes=[sqs[c]])
                            if c >= 1:
                                conv_stats(c - 1)
                        conv_stats(NKC - 1)
                        mu, rstd = ln_chain(p1c, p2c, NKC, MU, RSTD)
                        k.ps_pool = list(range(8))
                        for c in range(NKC):
                            y = scratch()
                            k.op("dve", lambda e, c=c, y=y: e.tensor_tensor(out=y[:], in0=CO[c][:], in1=mu[:], op=ALU.subtract),
                                 reads=[CO[c], mu], writes=[y])
                            k.op("dve", lambda e, y=y: e.tensor_tensor(out=y[:], in0=y[:], in1=rstd[:], op=ALU.mult),
                                 reads=[y, rstd], writes=[y])
                            k.op("act", lambda e, c=c, y=y: e.activation(out=CO[c][:], in_=y[:], func=AF.Silu,
                                                                        scale=PARL[:, c, P_LNCG:P_LNCG + 1],
                                                                        bias=PARL[:, c, P_LNCB:P_LNCB + 1]),
                                 reads=[y, PARL], writes=[CO[c]])
                        for half in range(2):
                            sgc = ws.get([slab_spec(win, 8192 + half * 512, 512, NKC)])
                            wg_ = wview(sgc, NKC, 512)
                            for j in range(4):
                                oc = half * 4 + j
                                pg = k.ps()
                                proj_fm(sgc, wg_, j, H1, pg)
                                k.op("act", lambda e, oc=oc, pg=pg: e.activation(out=SG[oc][:], in_=pg[:], func=AF.Sigmoid),
                                     reads=[pg], writes=[SG[oc]])
                            ws.release(1)
                        for half in range(2):
                            so = ws.get([slab_spec(w_conv_o[L], half * 512, 512, NKC)])
                            wo_ = wview(so, NKC, 512)
                            for j in range(4):
                                oc = half * 4 + j
                                py = k.ps()
                                proj_fm(so, wo_, j, CO, py)
                                k.op("dve", lambda e, oc=oc, py=py: e.tensor_tensor(out=M[oc][:], in0=py[:], in1=SG[oc][:], op=ALU.mult),
                                     reads=[py, SG[oc]], writes=[M[oc]])
                            ws.release(1)
                    k.barrier(("pe", "act", "dve", "sp"), bufs=prev_staging)
                    with ExitStack() as sp_:
                        VT = [[k.sb([128, DV], BF16, sp_) for _ in range(4)] for _ in range(2)]
                        KT = [[k.sb([128, DK], BF16, sp_) for _ in range(4)] for _ in range(2)]
                        RG = k.sb([128, 4, GT], BF16, sp_)
                        QD = [k.sb([128, 2, GT], BF16, sp_) for _ in range(2)]
                        PT = [k.sb([128, 4, 128], BF16, sp_) for _ in range(2)]
                        ON = [k.sb([128, DV], BF16, sp_) for _ in range(2)]
                        COS = k.sb([128, GT], F32, sp_)
                        SIN = k.sb([128, GT], F32, sp_)
                        SMALL = [k.sb([128, 64], F32, sp_) for _ in range(2)]
                        OG = [k.sb([128, 4, GT], BF16, sp_) for _ in range(HEADS)]
                        QK = [[k.sb([128, GT], BF16, sp_) for _ in range(4)] for _ in range(2)]
                        k.dma("sp", [(COS[:], cs_scr[0, :, cols])], reads=[CSB], writes=[COS])
                        k.dma("sp", [(SIN[:], cs_scr[1, :, cols])], reads=[CSB], writes=[SIN])
                        k.ps_pool = [0, 1, 2, 3]
                        PO = [k.psb[4 + i] for i in range(4)]

                        def A0(h):
                            qk = QK[h % 2]
                            sqk = ws.get([slab_spec(win, 2 * D + h * DK, DK, NKC, 0, 512),
                                          slab_spec(win, 3 * D + h * DK, DK, NKC, 256, 512)])
                            wqk = wview(sqk, NKC, 512)
                            for qi in range(2):
                                p1, p2 = k.ps(), k.ps()
                                proj_fm(sqk, wqk, qi * 2, H1, p1)
                                proj_fm(sqk, wqk, qi * 2 + 1, H1, p2)
                                t1, t2, t3, t4 = scratch(), scratch(), scratch(), scratch()
                                for (t, p, tab) in ((t1, p1, COS), (t2, p2, SIN), (t3, p2, COS), (t4, p1, SIN)):
                                    k.op("dve", lambda e, t=t, p=p, tab=tab: e.tensor_tensor(out=t[:], in0=p[:], in1=tab[:], op=ALU.mult),
                                         reads=[p, tab], writes=[t])
                                k.op("dve", lambda e, t1=t1, t2=t2, qi=qi: e.tensor_tensor(out=qk[qi * 2][:], in0=t1[:], in1=t2[:],
                                                                                          op=ALU.subtract),
                                     reads=[t1, t2], writes=[qk[qi * 2]])
                                k.op("dve", lambda e, t3=t3, t4=t4, qi=qi: e.tensor_tensor(out=qk[qi * 2 + 1][:], in0=t3[:], in1=t4[:],
                                                                                          op=ALU.add),
                                     reads=[t3, t4], writes=[qk[qi * 2 + 1]])
                            ws.release(1)

                        def A1(h):
                            vt = VT[h % 2]
                            sv = ws.get([slab_spec(win, 4 * D + h * DV, DV, NKC)])
                            wv_ = wview(sv, NKC, 512)
                            for tt in range(4):
                                pv = k.ps()
                                for kc in range(NKC):
                                    k.op("pe", lambda e, kc=kc, tt=tt, pv=pv: e.matmul(
                                        pv[:], lhsT=H1[kc][:, tt * 128:(tt + 1) * 128], rhs=wv_[:, kc, :],
                                        start=(kc == 0), stop=(kc == NKC - 1)),
                                        reads=[sv, H1[kc]], writes=[pv], inc=(kc == NKC - 1))
                                k.op("act", lambda e, tt=tt, pv=pv: e.activation(out=vt[tt][:], in_=pv[:], func=AF.Identity),
                                     reads=[pv], writes=[vt[tt]])
                            ws.release(1)

                        def A2(h):
                            srg = ws.get([slab_spec(win, 6 * D + h * DV, DV, NKC)])
                            wrg = wview(srg, NKC, 512)
                            for vc in range(4):
                                pr = k.ps()
                                proj_fm(srg, wrg, vc, H1, pr)
                                k.op("act", lambda e, vc=vc, pr=pr: e.activation(out=RG[:, vc, :], in_=pr[:], func=AF.Silu),
                                     reads=[pr], writes=[RG])
                            ws.release(1)

                        def A3(h):
                            qk, kt = QK[h % 2], KT[h % 2]
                            for tt in range(4):
                                pk = k.ps()
                                pkb = pk[:].bitcast(BF16)
                                for dc in range(2):
                                    k.op("pe", lambda e, tt=tt, dc=dc, pkb=pkb: e.transpose(
                                        out=pkb[:, dc * 128:(dc + 1) * 128], in_=qk[2 + dc][:, tt * 128:(tt + 1) * 128],
                                        identity=IDB[:]), reads=[qk[2 + dc], IDB], writes=[pk], inc=(dc == 1))
                                k.op("dve", lambda e, tt=tt, pkb=pkb, h=h: e.tensor_scalar(
                                    out=kt[tt][:], in0=pkb[:, 0:DK], scalar1=CON[:, C_KDEC + h:C_KDEC + h + 1], scalar2=None,
                                    op0=ALU.mult), reads=[pk, CON], writes=[kt[tt]])

                        def S123(h):
                            hb = h % 2
                            qk, vt, kt = QK[hb], VT[hb], KT[hb]
                            pt, qd, sm = PT[hb], QD[hb], SMALL[hb]
                            cdec = float(np.exp(np.float32(128.0) * np.log1p(np.float32(-2.0 ** (-5.0 - h)))))
                            mask = CON[:, C_MASK + h * 128:C_MASK + (h + 1) * 128]
                            qdec = CON[:, C_QDEC + h * 128:C_QDEC + (h + 1) * 128]
                            for dc in range(2):
                                k.op("act", lambda e, dc=dc: e.activation(out=STB[0][dc][:], in_=ST32[h][dc][:], func=AF.Identity),
                                     reads=[ST32[h][dc]], writes=[STB[0][dc]])
                            pss = k.ps()
                            for tt in range(4):
                                tc_ = slice(tt * 128, (tt + 1) * 128)
                                for dc in range(2):
                                    k.op("pe", lambda e, dc=dc, tc_=tc_: e.matmul(
                                        pss[:, tc_], lhsT=qk[2 + dc][:, tc_], rhs=qk[dc][:, tc_], start=(dc == 0), stop=(dc == 1)),
                                        reads=[qk[2 + dc], qk[dc]], writes=[pss], inc=(dc == 1 and tt == 3))
                            k.op("dve", lambda e: e.tensor_tensor(
                                out=pt[:], in0=pss[:].rearrange("p (t i) -> p t i", t=4),
                                in1=mask.unsqueeze(1).broadcast_to([128, 4, 128]), op=ALU.mult), reads=[pss, CON], writes=[pt])
                            for dc in range(2):
                                k.op("dve", lambda e, dc=dc: e.tensor_tensor(
                                    out=qd[:, dc, :].rearrange("p (t i) -> p t i", t=4), in0=qk[dc][:].rearrange("p (t i) -> p t i", t=4),
                                    in1=qdec.unsqueeze(1).broadcast_to([128, 4, 128]), op=ALU.mult), reads=[qk[dc], CON, qd], writes=[qd])
                            for tt in range(4):
                                tc_ = slice(tt * 128, (tt + 1) * 128)
                                sp_par, sn_par = tt % 2, (tt + 1) % 2
                                for dc in range(2):
                                    pd = k.ps()
                                    k.op("pe", lambda e, dc=dc, tt=tt, pd=pd: e.matmul(
                                        pd[:], lhsT=kt[tt][:, dc * 128:(dc + 1) * 128], rhs=vt[tt][:], start=True, stop=True),
                                        reads=[kt[tt], vt[tt]], writes=[pd])
                                    k.op("dve", lambda e, dc=dc, pd=pd: e.scalar_tensor_tensor(
                                        out=ST32[h][dc][:], in0=ST32[h][dc][:], scalar=cdec, in1=pd[:], op0=ALU.mult, op1=ALU.add),
                                        reads=[ST32[h][dc], pd], writes=[ST32[h][dc]])
                                    if tt < 3:
                                        k.op("act", lambda e, dc=dc, sn_par=sn_par: e.activation(out=STB[sn_par][dc][:], in_=ST32[h][dc][:],
                                                                                                func=AF.Identity),
                                             reads=[ST32[h][dc]], writes=[STB[sn_par][dc]])
                                po = PO[tt]
                                k.op("pe", lambda e, tt=tt, po=po: e.matmul(po[:], lhsT=pt[:, tt, :], rhs=vt[tt][:], start=True, stop=False),
                                     reads=[pt, vt[tt]], writes=[po], inc=False)
                                for dc in range(2):
                                    k.op("pe", lambda e, dc=dc, po=po, tc_=tc_, sp_par=sp_par: e.matmul(
                                        po[:], lhsT=qd[:, dc, tc_], rhs=STB[sp_par][dc][:], start=False, stop=(dc == 1)),
                                        reads=[qd, STB[sp_par][dc]], writes=[po], inc=(dc == 1))
                                k.op("dve", lambda e, tt=tt, po=po: e.bn_stats(out=sm[:, tt * 6:(tt + 1) * 6], in_=po[:]),
                                     reads=[po], writes=[sm])
                            for tt in range(4):
                                k.op("dve", lambda e, tt=tt: e.bn_aggr(out=sm[:, 24 + tt * 2:26 + tt * 2], in_=sm[:, tt * 6:(tt + 1) * 6]),
                                     reads=[sm], writes=[sm])
                            mv = sm[:, 24:32].rearrange("p (t two) -> p t two", two=2)
                            k.op("dve", lambda e: e.tensor_scalar(out=sm[:, 32:36], in0=mv[:, :, 1], scalar1=EPS, scalar2=None, op0=ALU.add),
                                 reads=[sm], writes=[sm])
                            k.op("act", lambda e: e.activation(out=sm[:, 36:40], in_=sm[:, 32:36], func=AF.Sqrt), reads=[sm], writes=[sm])
                            k.op("dve", lambda e: e.reciprocal(out=sm[:, 40:44], in_=sm[:, 36:40]), reads=[sm], writes=[sm])
                            k.op("dve", lambda e: e.scalar_tensor_tensor(out=sm[:, 44:48], in0=mv[:, :, 0], scalar=-1.0, in1=sm[:, 40:44],
                                                                          op0=ALU.mult, op1=ALU.mult), reads=[sm], writes=[sm])

                        def S4(h):
                            sm = SMALL[h % 2]

                            def evac(tt):
                                on, po = ON[tt % 2], PO[tt]
                                k.op("act", lambda e, on=on, po=po, tt=tt: e.activation(out=on[:], in_=po[:], func=AF.Identity,
                                                                                       scale=sm[:, 40 + tt:41 + tt], bias=sm[:, 44 + tt:45 + tt]),
                                     reads=[po, sm], writes=[on])
                            evac(0)
                            evac(1)
                            for tt in range(4):
                                tc_ = slice(tt * 128, (tt + 1) * 128)
                                on = ON[tt % 2]
                                ptb = k.ps()
                                ptbb = ptb[:].bitcast(BF16)
                                for vc in range(4):
                                    k.op("pe", lambda e, vc=vc, on=on, ptbb=ptbb: e.transpose(
                                        out=ptbb[:, vc * 128:(vc + 1) * 128], in_=on[:, vc * 128:(vc + 1) * 128], identity=IDB[:]),
                                        reads=[on, IDB], writes=[ptb], inc=(vc == 3))
                                if tt + 2 < 4:
                                    evac(tt + 2)
                                k.op("dve", lambda e, tc_=tc_, ptbb=ptbb: e.tensor_tensor(
                                    out=OG[h][:, :, tc_], in0=ptbb[:, 0:512].rearrange("p (v t) -> p v t", v=4), in1=RG[:, :, tc_],
                                    op=ALU.mult), reads=[ptb, RG], writes=[OG[h]])

                        A0(0); A1(0); A2(0); A3(0)
                        for h in range(HEADS):
                            S123(h)
                            if h + 1 < HEADS:
                                A0(h + 1)
                                A1(h + 1)
                            S4(h)
                            if h + 1 < HEADS:
                                A2(h + 1)
                                A3(h + 1)
                        for q4 in range(4):
                            sr = ws.get([slab_spec(w_ret_o[L], q4 * 256, 256, 16)])
                            wr_ = wview(sr, 16, 256)
                            if q4 % 2 == 0:
                                sgr = ws.get([slab_spec(win, 9 * D + (q4 // 2) * 512, 512, NKC)])
                                wgr = wview(sgr, NKC, 512)
                            for j in range(2):
                                oc = q4 * 2 + j
                                py, pg = k.ps(), k.ps()
                                for kc in range(16):
                                    k.op("pe", lambda e, kc=kc, j=j, py=py: e.matmul(
                                        py[:], lhsT=wr_[:, kc, j * 128:(j + 1) * 128], rhs=OG[kc // 4][:, kc % 4, :],
                                        start=(kc == 0), stop=(kc == 15)), reads=[sr, OG[kc // 4]], writes=[py], inc=(kc == 15))
                                proj_fm(sgr, wgr, (q4 % 2) * 2 + j, H1, pg)
                                sg, tm = scratch(), scratch()
                                k.op("act", lambda e, sg=sg, pg=pg: e.activation(out=sg[:], in_=pg[:], func=AF.Sigmoid),
                                     reads=[pg], writes=[sg])
                                k.op("dve", lambda e, sg=sg, py=py, tm=tm: e.tensor_tensor(out=tm[:], in0=py[:], in1=sg[:], op=ALU.mult),
                                     reads=[py, sg], writes=[tm])
                                k.op("dve", lambda e, oc=oc, tm=tm: e.tensor_tensor(out=M[oc][:], in0=M[oc][:], in1=tm[:], op=ALU.add),
                                     reads=[M[oc], tm], writes=[M[oc]])
                            ws.release(1)
                            if q4 % 2 == 1:
                                ws.release(1)
                        snap = k.snapshot()
                        if g + 1 < NG:
                            emit_h1(g + 1)
                        for half in range(2):
                            so = ws.get([slab_spec(w_out[L], half * 512, 512, NKC)])
                            wo_ = wview(so, NKC, 512)
                            for j in range(4):
                                oc = half * 4 + j
                                pm = k.ps()
                                proj_fm(so, wo_, j, M, pm)
                                k.op("dve", lambda e, oc=oc, pm=pm: e.scalar_tensor_tensor(
                                    out=XA[oc][:, cols], in0=pm[:], scalar=V[:, V_G1, oc:oc + 1], in1=XA[oc][:, cols],
                                    op0=ALU.mult, op1=ALU.add), reads=[pm, V, XA[oc]], writes=[XA[oc]])
                            ws.release(1)
                        mu, rstd = ln_stats(scratch, [(XA[c], XA[c][:, cols]) for c in range(NKC)], MU, RSTD, False)
                        for c in range(NKC):
                            y = scratch()
                            k.op("dve", lambda e, c=c, y=y: e.tensor_tensor(out=y[:], in0=XA[c][:, cols], in1=mu[:], op=ALU.subtract),
                                 reads=[XA[c], mu], writes=[y])
                            k.op("dve", lambda e, y=y: e.tensor_tensor(out=y[:], in0=y[:], in1=rstd[:], op=ALU.mult),
                                 reads=[y, rstd], writes=[y])
                            k.op("act", lambda e, c=c, y=y: e.activation(out=XA[c][:, cols], in_=y[:], func=AF.Identity,
                                                                        scale=V[:, V_XS1, c:c + 1], bias=V[:, V_XB1, c:c + 1]),
                                 reads=[y, V], writes=[XA[c]])
                            hh = QK[0][c % 4] if c < 4 else QK[1][c % 4]
                            k.op("act", lambda e, c=c, y=y, hh=hh: e.activation(out=hh[:], in_=y[:], func=AF.Identity,
                                                                               scale=V[:, V_GS2, c:c + 1], bias=V[:, V_BS2, c:c + 1]),
                                 reads=[y, V], writes=[hh])
                            k.dma("sp", [(h2_scr[c, :, cols], hh[:])], reads=[hh], writes=[H2B[c]])
                        if g == NG - 1:
                            k.barrier(("pe", "act", "dve", "sp", "pool"), bufs=QK[0] + QK[1])
                        else:
                            prev_snap, prev_staging = snap, QK[0] + QK[1]
                    k.ps_pool = list(range(8))

        def ffn(li, l):
            L = lw(l)
            V = VEC[li]
            moe = (l % 2 == 1)
            i2 = (l // 2) if fused else 0
            with ExitStack() as ph:
                H2 = [k.sb([128, T], BF16, ph) for _ in range(NKC)]
                A = [[k.sb([128, T], BF16, ph) for _ in range(FB)] for _ in range(2)]
                scr = [k.sb([128, GT], F32, ph) for _ in range(3)]
                MU = k.sb([128, GT], F32, ph)
                RSTD = k.sb([128, GT], F32, ph)
                scr_i = [0]

                def scratch():
                    b = scr[scr_i[0] % len(scr)]
                    scr_i[0] += 1
                    return b
                for c in range(NKC):
                    k.dma("sp", [(H2[c][:], h2_scr[c])], reads=[H2B[c]], writes=[H2[c]])
                if moe:
                    CMB = k.sb([128, 16, NE], F32, ph)
                    CB = [k.sb([128, T], BF16, ph) for _ in range(2)]
                    SM = [k.sb([128, 64], F32, ph) for _ in range(2)]
                    DGF = [k.sb([128, 128], F32, ph) for _ in range(2)]
                    for tt in range(16):
                        tc_ = slice(tt * 128, (tt + 1) * 128)
                        sm = SM[tt % 2]
                        pl = k.ps()
                        for kc in range(NKC):
                            k.op("pe", lambda e, kc=kc, tc_=tc_, pl=pl: e.matmul(pl[:, 0:NE], lhsT=H2[kc][:, tc_], rhs=WRB[:, i2, kc, :],
                                                                                start=(kc == 0), stop=(kc == NKC - 1)),
                                 reads=[H2[kc], WRB], writes=[pl], inc=(kc == NKC - 1))
                        lg, m1, mk1, l2, m2, mk2, w1, w2 = (sm[:, 0:8], sm[:, 8:9], sm[:, 16:24], sm[:, 24:32], sm[:, 9:10],
                                                            sm[:, 32:40], sm[:, 10:11], sm[:, 11:12])

                        def sop(fn, eng="dve", sm=sm, extra=()):
                            k.op(eng, fn, reads=[sm, *extra], writes=[sm])
                        k.op("dve", lambda e, lg=lg, pl=pl: e.tensor_copy(out=lg, in_=pl[:, 0:NE]), reads=[pl], writes=[sm])
                        sop(lambda e, lg=lg, m1=m1: e.tensor_reduce(out=m1, in_=lg, axis=mybir.AxisListType.X, op=ALU.max))
                        sop(lambda e, lg=lg, m1=m1, mk1=mk1: e.tensor_scalar(out=mk1, in0=lg, scalar1=m1, scalar2=None, op0=ALU.is_ge))
                        sop(lambda e, lg=lg, mk1=mk1, l2=l2: e.scalar_tensor_tensor(out=l2, in0=mk1, scalar=-1e30, in1=lg,
                                                                                   op0=ALU.mult, op1=ALU.add))
                        sop(lambda e, l2=l2, m2=m2: e.tensor_reduce(out=m2, in_=l2, axis=mybir.AxisListType.X, op=ALU.max))
                        sop(lambda e, l2=l2, m2=m2, mk2=mk2: e.tensor_scalar(out=mk2, in0=l2, scalar1=m2, scalar2=None, op0=ALU.is_ge))
                        sop(lambda e, m1=m1, m2=m2, w2=w2: e.tensor_tensor(out=w2, in0=m2, in1=m1, op=ALU.subtract))
                        sop(lambda e, w2=w2: e.activation(out=w2, in_=w2, func=AF.Sigmoid), eng="act")
                        sop(lambda e, w1=w1, w2=w2: e.tensor_scalar(out=w1, in0=w2, scalar1=-1.0, scalar2=1.0, op0=ALU.mult, op1=ALU.add))
                        sop(lambda e, mk1=mk1, w1=w1: e.tensor_scalar(out=mk1, in0=mk1, scalar1=w1, scalar2=None, op0=ALU.mult))
                        k.op("dve", lambda e, mk1=mk1, mk2=mk2, w2=w2, tt=tt: e.scalar_tensor_tensor(
                            out=CMB[:, tt, :], in0=mk2, scalar=w2, in1=mk1, op0=ALU.mult, op1=ALU.add), reads=[sm], writes=[CMB])

                nexp = NE if moe else 1
                blocks = [(b0, min(FB, NFC - b0)) for b0 in range(0, NFC, FB)]
                bi = 0
                ada_tasks = []
                if li + 1 < NL:
                    ln_ = layers[li + 1]
                    PARN = k.sb([128, NKC, NP_], F32, ph)
                    ASL = [k.sb([128, NKC, 512], BF16, ph) for _ in range(2)]
                    ADAN = k.sb([128, 48], F32, ph)
                    k.dma("sp", [(PARN[:], params.rearrange("p (l c n) -> p l c n", l=LW, c=NKC)[:, lw(ln_)])], writes=[PARN])
                    wan = w_ada[lw(ln_)].rearrange("(kc p) m -> p kc m", p=128)
                    pan = k.psb[7]
                    k.ps_pool = list(range(7))
                    k.barrier(("pool",))
                    k.dma("pool", [(ASL[0][:], wan[:, :, 0:512])], writes=[ASL[0]])

                    def ada_task(j):
                        if j + 1 < 12:
                            k.dma("pool", [(ASL[(j + 1) % 2][:], wan[:, :, (j + 1) * 512:(j + 2) * 512])], writes=[ASL[(j + 1) % 2]])
                        slab = ASL[j % 2]
                        for jj in range(4):
                            oc = j * 4 + jj
                            for kc in range(NKC):
                                k.op("pe", lambda e, jj=jj, kc=kc, oc=oc: e.matmul(
                                    pan[:, oc:oc + 1], lhsT=slab[:, kc, jj * 128:(jj + 1) * 128], rhs=CAB[:, kc:kc + 1],
                                    start=(kc == 0), stop=(kc == NKC - 1)), reads=[slab, CAB], writes=[pan], inc=(kc == NKC - 1))
                        if j == 11:
                            compute_vec(li + 1, ln_, pan, ADAN, PARN, lambda c: PARN[:, :, c])
                    ada_tasks = list(range(12))
                    per_block = 2 if not moe else 1
                for ex in range(nexp):
                    if moe:
                        wgm, wum, wdm = moe_wg[i2, ex], moe_wu[i2, ex], moe_wd[i2, ex]
                        cb = CB[ex % 2]
                        for tt in range(16):
                            dg = DGF[tt % 2]
                            k.op("dve", lambda e, dg=dg, tt=tt, ex=ex: e.tensor_scalar(out=dg[:], in0=ident_f, scalar1=CMB[:, tt, ex:ex + 1],
                                                                                      scalar2=None, op0=ALU.mult),
                                 reads=[CON, CMB], writes=[dg])
                            if tt % 4 == 0:
                                pcb = k.ps()
                            k.op("pe", lambda e, dg=dg, tt=tt, pcb=pcb: e.matmul(pcb[:, (tt % 4) * 128:(tt % 4 + 1) * 128], lhsT=ones_f,
                                                                                rhs=dg[:], start=True, stop=True),
                                 reads=[CON, dg], writes=[pcb])
                            if tt % 4 == 3:
                                k.op("act", lambda e, cb=cb, tt=tt, pcb=pcb: e.activation(out=cb[:, (tt // 4) * GT:(tt // 4 + 1) * GT], in_=pcb[:], func=AF.Identity),
                                     reads=[pcb], writes=[cb])
                    else:
                        wgm, wum, wdm = ffn_wg[i2], ffn_wu[i2], ffn_wd[i2]
                    for (b0, nb) in blocks:
                        Ab = A[bi % 2]
                        bi += 1
                        sg_ = ws.get([slab_spec(wgm, b0 * 128, nb * 128, NKC, 0, 512)])
                        su_ = ws.get([slab_spec(wum, b0 * 128, nb * 128, NKC, 0, 512)])
                        wdsrc = wdm[b0 * 128:(b0 + nb) * 128, :].rearrange("(f p) m -> p f m", p=128)
                        sd_ = ws.get([(lambda t, nb=nb: t[:, 0:nb * D].rearrange("p (f m) -> p f m", f=nb), wdsrc)])
                        wg_, wu_ = wview(sg_, NKC, 512), wview(su_, NKC, 512)
                        wd_ = sd_.t[:, 0:nb * D].rearrange("p (f m) -> p f m", f=nb)
                        for f in range(nb):
                            for g in range(NG):
                                cols = slice(g * GT, (g + 1) * GT)
                                pg, pu = k.ps(), k.ps()
                                for kc in range(NKC):
                                    k.op("pe", lambda e, kc=kc, f=f, pg=pg, cols=cols: e.matmul(
                                        pg[:], lhsT=wg_[:, kc, f * 128:(f + 1) * 128], rhs=H2[kc][:, cols],
                                        start=(kc == 0), stop=(kc == NKC - 1)), reads=[sg_, H2[kc]], writes=[pg], inc=(kc == NKC - 1))
                                for kc in range(NKC):
                                    k.op("pe", lambda e, kc=kc, f=f, pu=pu, cols=cols: e.matmul(
                                        pu[:], lhsT=wu_[:, kc, f * 128:(f + 1) * 128], rhs=H2[kc][:, cols],
                                        start=(kc == 0), stop=(kc == NKC - 1)), reads=[su_, H2[kc]], writes=[pu], inc=(kc == NKC - 1))
                                sgs = scratch()
                                k.op("act", lambda e, sgs=sgs, pg=pg: e.activation(out=sgs[:], in_=pg[:], func=AF.Silu),
                                     reads=[pg], writes=[sgs])
                                if moe:
                                    k.op("dve", lambda e, sgs=sgs, cb=cb, cols=cols: e.tensor_tensor(out=sgs[:], in0=sgs[:], in1=cb[:, cols],
                                                                                                    op=ALU.mult),
                                         reads=[sgs, cb], writes=[sgs])
                                k.op("dve", lambda e, sgs=sgs, pu=pu, f=f, cols=cols, Ab=Ab: e.tensor_tensor(
                                    out=Ab[f][:, cols], in0=pu[:], in1=sgs[:], op=ALU.mult), reads=[pu, sgs], writes=[Ab[f]])
                        ws.release(2)
                        for _ in range(per_block if ada_tasks else 0):
                            if ada_tasks:
                                ada_task(ada_tasks.pop(0))
                        for oc in range(NKC):
                            for g in range(NG):
                                cols = slice(g * GT, (g + 1) * GT)
                                pd = k.ps()
                                for f in range(nb):
                                    k.op("pe", lambda e, f=f, oc=oc, pd=pd, cols=cols, Ab=Ab: e.matmul(
                                        pd[:], lhsT=wd_[:, f, oc * 128:(oc + 1) * 128], rhs=Ab[f][:, cols],
                                        start=(f == 0), stop=(f == nb - 1)), reads=[sd_, Ab[f]], writes=[pd], inc=(f == nb - 1))
                                k.op("dve", lambda e, oc=oc, pd=pd, cols=cols: e.scalar_tensor_tensor(
                                    out=XA[oc][:, cols], in0=pd[:], scalar=V[:, V_G2, oc:oc + 1], in1=XA[oc][:, cols],
                                    op0=ALU.mult, op1=ALU.add), reads=[pd, V, XA[oc]], writes=[XA[oc]])
                        ws.release(1)
                assert not ada_tasks
                for g in range(NG):
                    cols = slice(g * GT, (g + 1) * GT)
                    mu, rstd = ln_stats(scratch, [(XA[c], XA[c][:, cols]) for c in range(NKC)], MU, RSTD, False)
                    for c in range(NKC):
                        k.op("dve", lambda e, c=c, cols=cols, mu=mu: e.tensor_tensor(out=XA[c][:, cols], in0=XA[c][:, cols], in1=mu[:],
                                                                                    op=ALU.subtract),
                             reads=[XA[c], mu], writes=[XA[c]])
                        k.op("dve", lambda e, c=c, cols=cols, rstd=rstd: e.tensor_tensor(out=XA[c][:, cols], in0=XA[c][:, cols], in1=rstd[:],
                                                                                        op=ALU.mult),
                             reads=[XA[c], rstd], writes=[XA[c]])
                        k.op("act", lambda e, c=c, cols=cols: e.activation(out=XA[c][:, cols], in_=XA[c][:, cols], func=AF.Identity,
                                                                          scale=V[:, V_XS2, c:c + 1], bias=V[:, V_XB2, c:c + 1]),
                             reads=[XA[c], V], writes=[XA[c]])

        k.dry = True
        emit()
        k.dry = False
        k.psi = 0
        ws.reset()
        emit()
    return nc


def _consts():
    c = np.zeros((128, C_END), np.float32)
    c[:, C_ID:C_ID + 128] = np.eye(128, dtype=np.float32)
    c[:, C_ONES:C_ONES + 128] = 1.0
    j = np.arange(128, dtype=np.float32)
    for h in range(HEADS):
        log_g = np.log1p(np.float32(-np.exp2(np.float32(-5.0 - h)))).astype(np.float32)
        diff = j[None, :] - j[:, None]
        m = np.where(diff >= 0, np.exp(log_g * np.maximum(diff, 0.0)), 0.0).astype(np.float32)
        c[:, C_MASK + h * 128:C_MASK + (h + 1) * 128] = m * np.float32(DK ** -0.5)
        c[:, C_QDEC + h * 128:C_QDEC + (h + 1) * 128] = np.exp(log_g * (j + 1.0)).astype(np.float32)[None, :]
        c[:, C_KDEC + h] = np.exp(log_g * (127.0 - j)).astype(np.float32) * np.float32(DK ** -0.5)
    half = 128
    c[:, C_INVF] = (10000.0 ** (-np.arange(half, dtype=np.float32) / half)).astype(np.float32)
    return c


def _params(inp, ls):
    out = np.zeros((128, len(ls), NKC, NP_), np.float32)
    for i, l in enumerate(ls):
        rows = [inp["b_dw"][l], inp["ln_conv_g"][l], inp["ln_conv_b"][l], inp["ln1_g"][l], inp["ln1_b"][l],
                inp["ln2_g"][l], inp["ln2_b"][l]] + [inp["w_dw"][l][t] for t in range(CK)] + \
               [inp["b_ada"][l].reshape(6, D)[j] for j in range(6)]
        m = np.stack(rows, 0).reshape(NP_, NKC, 128)
        out[:, i] = m.transpose(2, 1, 0)
    return np.ascontiguousarray(out.reshape(128, -1))


_PROG = {}


def _get_prog(key, layers, fused):
    if key not in _PROG:
        _PROG[key] = build_program(layers, fused)
    return _PROG[key]


FUSED = True


def kernel(**inp):
    inp = {k_: np.asarray(v) for k_, v in inp.items()}
    B = inp["x"].shape[0]
    consts = _consts()
    cores = list(range(B))
    xT = [np.ascontiguousarray(inp["x"][b].T) for b in range(B)]
    cTs = [np.ascontiguousarray(inp["c"][b].reshape(NKC, 128).T) for b in range(B)]
    posb = [np.ascontiguousarray(np.broadcast_to(inp["positions"][b].astype(np.int32)[None, :], (128, T))) for b in range(B)]

    def wr_layout(ws_):
        n = ws_.shape[0]
        return np.ascontiguousarray(ws_.reshape(n, NKC, 128, NE).transpose(2, 0, 1, 3).reshape(128, -1))

    if FUSED:
        nc = _get_prog("fused", list(range(DEPTH)), True)
        shared = dict(params=_params(inp, list(range(DEPTH))), consts=consts, w_ada=inp["w_ada"], w_in=inp["w_in"],
                      w_conv_o=inp["w_conv_o"], w_ret_o=inp["w_ret_o"], w_out=inp["w_out"],
                      ffn_w_gate=inp["ffn_w_gate"], ffn_w_up=inp["ffn_w_up"], ffn_w_down=inp["ffn_w_down"],
                      moe_wr=wr_layout(inp["moe_w_router"]), moe_w_gate=inp["moe_w_gate"], moe_w_up=inp["moe_w_up"],
                      moe_w_down=inp["moe_w_down"])
        in_maps = [dict(shared, xT=xT[b], cT=cTs[b], posb=posb[b]) for b in range(B)]
        res = run_bass_kernel_spmd(nc, in_maps, core_ids=cores)
        outs = [res.results[b]["outT"] for b in range(B)]
    else:
        cur = xT
        for l in range(DEPTH):
            moe = (l % 2 == 1)
            nc = _get_prog("moe" if moe else "dense", [l % 2], False)
            i2 = l // 2
            shared = dict(params=_params(inp, [l]), consts=consts, w_ada=inp["w_ada"][l:l + 1], w_in=inp["w_in"][l:l + 1],
                          w_conv_o=inp["w_conv_o"][l:l + 1], w_ret_o=inp["w_ret_o"][l:l + 1], w_out=inp["w_out"][l:l + 1])
            if moe:
                shared.update(moe_wr=wr_layout(inp["moe_w_router"][i2:i2 + 1]), moe_w_gate=inp["moe_w_gate"][i2:i2 + 1],
                              moe_w_up=inp["moe_w_up"][i2:i2 + 1], moe_w_down=inp["moe_w_down"][i2:i2 + 1])
            else:
                shared.update(ffn_w_gate=inp["ffn_w_gate"][i2:i2 + 1], ffn_w_up=inp["ffn_w_up"][i2:i2 + 1],
                              ffn_w_down=inp["ffn_w_down"][i2:i2 + 1])
            in_maps = [dict(shared, xT=cur[b], cT=cTs[b], posb=posb[b]) for b in range(B)]
            res = run_bass_kernel_spmd(nc, in_maps, core_ids=cores)
            cur = [res.results[b]["outT"] for b in range(B)]
        outs = cur
    return np.stack([o.T for o in outs], 0).astype(np.float32)
```
